# Optimizing a Trainium2 kernel written in Bass

```python
import math
import jax, jax.numpy as jnp
from jax import lax
import numpy as np

D_MODEL = 1024
BATCH = 8
SEQ = 8192
DEPTH = 2

GRID_W = 64
CTX_LEN = 256
EPS = 1e-6

DA_HEADS = 4
DA_HEAD_DIM = 64
DA_V_DIM = 2 * DA_HEAD_DIM
DA_QK_W = DA_HEADS * 2 * DA_HEAD_DIM
DA_V_W = DA_HEADS * DA_V_DIM
Q_BLOCK = 128
ROPE_BASE = 10000.0

SC_W = 512
SC_K = 3

FN_W = 512
FN_GROUPS = 4

POOL_WINDOWS = (2, 4, 8, 16)
POOL_W = 512
POOL_G = len(POOL_WINDOWS)
POOL_GW = POOL_W // POOL_G

N_BRANCH = 4
BRANCH_W = 512

COL_WIDTHS = (DA_QK_W, DA_QK_W, DA_V_W, SC_W, SC_W, SC_W, FN_W, POOL_W)
IN_W = sum(COL_WIDTHS)
COL_SPLITS = tuple(sum(COL_WIDTHS[:i + 1]) for i in range(len(COL_WIDTHS) - 1))
KV_LO = DA_QK_W
KV_HI = 2 * DA_QK_W + DA_V_W

N_EXPERTS = 32
TOP_K = 4
D_EXPERT = D_MODEL
SWIGLU_LIMIT = 7.0
SWIGLU_ALPHA = 1.702

kernel_name = "hybrid_parallel_diffusion_block"


def rmsnorm(x, g):
    xf = x.astype(jnp.float32)
    y = xf * lax.rsqrt(jnp.mean(xf * xf, axis=-1, keepdims=True) + EPS)
    return (y * g.astype(jnp.float32)).astype(x.dtype)


def adaln(cv, w, b):
    m = jax.nn.silu(cv) @ w + b
    return m.reshape(m.shape[:-1] + (6, w.shape[0]))


def modulate(h, shift, scale):
    return h * (1 + scale) + shift


def axial_rope(n):
    rows = n // GRID_W
    row = jnp.repeat(jnp.arange(rows), GRID_W).astype(jnp.float32)
    col = jnp.tile(jnp.arange(GRID_W), rows).astype(jnp.float32)
    half = DA_HEAD_DIM // 2
    inv = ROPE_BASE ** (-jnp.arange(0, half, 2, dtype=jnp.float32) / half)
    ar = row[:, None] * inv
    ac = col[:, None] * inv
    ang = jnp.concatenate([ar, ar, ac, ac], axis=-1)
    return jnp.cos(ang), jnp.sin(ang)


def apply_rope(x, cos, sin):
    x0, x1, x2, x3 = jnp.split(x, 4, axis=-1)
    rot = jnp.concatenate([-x1, x0, -x3, x2], axis=-1)
    return x * cos + rot * sin


def split_qk(t):
    b, n, _ = t.shape
    return t.reshape(b, n, DA_HEADS, 2, DA_HEAD_DIM).transpose(0, 2, 3, 1, 4)


def split_v(t):
    b, n, _ = t.shape
    return t.reshape(b, n, DA_HEADS, DA_V_DIM).transpose(0, 2, 1, 3)


def diff_lambda(lam, lam_init):
    lf = lam.astype(jnp.float32)
    return jnp.exp(jnp.sum(lf[0] * lf[1])) - jnp.exp(jnp.sum(lf[2] * lf[3])) + lam_init


def diff_attend(q, k, v, lam):
    s = jnp.einsum('bhiqd,bhikd->bhiqk', q, k).astype(jnp.float32) * (DA_HEAD_DIM ** -0.5)
    p = jax.nn.softmax(s, axis=-1)
    a = p[:, :, 0] - lam * p[:, :, 1]
    return jnp.einsum('bhqk,bhkv->bhqv', a.astype(v.dtype), v)


def latent_diff_attention(q, k_all, v_all, lam):
    b, h, _, s, d = q.shape
    nb = s // Q_BLOCK
    qb = q.reshape(b, h, 2, nb, Q_BLOCK, d).transpose(3, 0, 1, 2, 4, 5)
    ob = lax.map(lambda qq: diff_attend(qq, k_all, v_all, lam), qb)
    return ob.transpose(1, 2, 0, 3, 4).reshape(b, h, s, DA_V_DIM)


def diff_head_out(o, g_sub, lam_init):
    o = rmsnorm(o, g_sub) * (1 - lam_init)
    b, h, n, dv = o.shape
    return o.transpose(0, 2, 1, 3).reshape(b, n, h * dv)


def short_conv(xin, bgate, cgate, w):
    u = cgate * xin
    up = jnp.pad(u, ((0, 0), (1, 1), (0, 0)))
    y = w[0] * up[:, :-2] + w[1] * up[:, 1:-1] + w[2] * up[:, 2:]
    return bgate * y


def fourier_mix(xf):
    b, n, w = xf.shape
    g = xf.astype(jnp.float32).reshape(b, n, FN_GROUPS, w // FN_GROUPS)
    y = jnp.fft.fftn(g, axes=(1, 3), norm='ortho').real
    return y.reshape(b, n, w).astype(xf.dtype)


def multiscale_pool(xp, w_grp, scale):
    b, n, w = xp.shape
    xf = xp.reshape(b, n, POOL_G, POOL_GW).astype(jnp.float32)
    cs = jnp.concatenate([jnp.zeros_like(xf[:, :1]), jnp.cumsum(xf, axis=1)], axis=1)
    t = jnp.arange(n)[:, None]
    half = jnp.array([wd // 2 for wd in POOL_WINDOWS])[None, :]
    lo = jnp.clip(t - half, 0, n)
    hi = jnp.clip(t + half, 0, n)
    gi = jnp.arange(POOL_G)[None, :]
    win_sum = cs[:, hi, gi] - cs[:, lo, gi]
    cnt = (hi - lo).astype(jnp.float32)[None, :, :, None]
    pooled = (win_sum / cnt - xf).astype(xp.dtype)
    y = jnp.einsum('bngc,gcd->bngd', pooled, w_grp)
    return y.reshape(b, n, w) * scale


def merge_branches(ys, h, w_branch, w_gate, b_gate, w_out):
    p = jnp.einsum('bnkc,kcd->bnkd', ys, w_branch)
    g = jax.nn.sigmoid(h @ w_gate + b_gate).reshape(p.shape)
    return jnp.sum(g * p, axis=2) @ w_out


def moe_ffn(h, w_router, b_router, w_g, b_g, w_u, b_u, w_d, b_d):
    shp = h.shape
    t = h.reshape(-1, shp[-1])
    logits = (t @ w_router + b_router).astype(jnp.float32)
    top_v, top_i = lax.top_k(logits, TOP_K)
    top_w = jax.nn.softmax(top_v, axis=-1)
    comb = jnp.sum(jax.nn.one_hot(top_i, N_EXPERTS, dtype=jnp.float32) * top_w[..., None], axis=1)
    comb = comb.astype(h.dtype)

    def expert_step(acc, ex):
        wg, bg, wu, bu, wd, bd, ce = ex
        a = jnp.minimum(t @ wg + bg, SWIGLU_LIMIT)
        u = jnp.clip(t @ wu + bu, -SWIGLU_LIMIT, SWIGLU_LIMIT)
        y = (a * jax.nn.sigmoid(SWIGLU_ALPHA * a) * (u + 1)) @ wd + bd
        return acc + ce[:, None] * y, None

    acc, _ = lax.scan(expert_step, jnp.zeros_like(t), (w_g, b_g, w_u, b_u, w_d, b_d, comb.T))
    return acc.reshape(shp)


def setup_inputs(seed: int = 0) -> dict:
    key = jax.random.key(seed)
    ks = jax.random.split(key, 32)
    f32 = jnp.float32

    def nrm(k, shape, s):
        return jax.random.normal(k, shape, f32) * s

    L, D, E, F = DEPTH, D_MODEL, N_EXPERTS, D_EXPERT
    return {
        "x": nrm(ks[0], (BATCH, SEQ, D), 1.0),
        "c": nrm(ks[1], (BATCH, D), 1.0),
        "ctx": nrm(ks[2], (BATCH, CTX_LEN, D), 1.0),
        "c_ctx": nrm(ks[3], (D,), 1.0),
        "w_ada": nrm(ks[4], (L, D, 6 * D), 0.5 * D ** -0.5),
        "b_ada": nrm(ks[5], (L, 6 * D), 0.02),
        "g_norm1": 1.0 + nrm(ks[6], (L, D), 0.05),
        "w_in": nrm(ks[7], (L, D, IN_W), D ** -0.5),
        "da_lambda": nrm(ks[8], (L, 4, DA_HEAD_DIM), 0.1),
        "g_subln": 1.0 + nrm(ks[9], (L, DA_V_DIM), 0.05),
        "w_conv": nrm(ks[10], (L, SC_K, SC_W), SC_K ** -0.5),
        "w_pool": nrm(ks[11], (L, POOL_G, POOL_GW, POOL_GW), POOL_GW ** -0.5),
        "pool_scale": 1.0 + nrm(ks[12], (L, POOL_W), 0.1),
        "w_branch": nrm(ks[13], (L, N_BRANCH, BRANCH_W, D), BRANCH_W ** -0.5),
        "w_mgate": nrm(ks[14], (L, D, N_BRANCH * D), D ** -0.5),
        "b_mgate": nrm(ks[15], (L, N_BRANCH * D), 0.02),
        "w_out": nrm(ks[16], (L, D, D), D ** -0.5),
        "g_norm2": 1.0 + nrm(ks[17], (L, D), 0.05),
        "w_router": nrm(ks[18], (L, D, E), D ** -0.5),
        "b_router": nrm(ks[19], (L, E), 0.01),
        "w_e_gate": nrm(ks[20], (L, E, D, F), D ** -0.5),
        "b_e_gate": nrm(ks[21], (L, E, F), 0.01),
        "w_e_up": nrm(ks[22], (L, E, D, F), D ** -0.5),
        "b_e_up": nrm(ks[23], (L, E, F), 0.01),
        "w_e_down": nrm(ks[24], (L, E, F, D), F ** -0.5),
        "b_e_down": nrm(ks[25], (L, E, D), 0.01),
        "g_final": 1.0 + nrm(ks[26], (D,), 0.05),
    }


def reference(x, c, ctx, c_ctx, w_ada, b_ada, g_norm1, w_in, da_lambda, g_subln, w_conv, w_pool,
              pool_scale, w_branch, w_mgate, b_mgate, w_out, g_norm2, w_router, b_router,
              w_e_gate, b_e_gate, w_e_up, b_e_up, w_e_down, b_e_down, g_final):
    dt = x.dtype
    n_lat = x.shape[1]
    cos, sin = axial_rope(n_lat)
    cos = cos.astype(dt)
    sin = sin.astype(dt)

    for l in range(DEPTH):
        last = l == DEPTH - 1
        lam_init = 0.8 - 0.6 * math.exp(-0.3 * l)
        lam = diff_lambda(da_lambda[l], lam_init)
        m_lat = adaln(c, w_ada[l], b_ada[l])[:, None]
        m_ctx = adaln(c_ctx, w_ada[l], b_ada[l])

        h_lat = modulate(rmsnorm(x, g_norm1[l]), m_lat[..., 0, :], m_lat[..., 1, :])
        h_ctx = modulate(rmsnorm(ctx, g_norm1[l]), m_ctx[0], m_ctx[1])

        q, k, v, sx, sb, sc, fx, px = jnp.split(h_lat @ w_in[l], COL_SPLITS, axis=-1)
        if last:
            ck, cv = jnp.split(h_ctx @ w_in[l][:, KV_LO:KV_HI], [DA_QK_W], axis=-1)
        else:
            cq, ck, cv, csx, csb, csc, cfx, cpx = jnp.split(h_ctx @ w_in[l], COL_SPLITS, axis=-1)

        ck_h = split_qk(ck)
        cv_h = split_v(cv)
        q_lat = apply_rope(split_qk(q), cos, sin)
        k_all = jnp.concatenate([ck_h, apply_rope(split_qk(k), cos, sin)], axis=3)
        v_all = jnp.concatenate([cv_h, split_v(v)], axis=2)
        a_lat = diff_head_out(latent_diff_attention(q_lat, k_all, v_all, lam), g_subln[l], lam_init)

        y_lat = jnp.stack([
            a_lat,
            short_conv(sx, sb, sc, w_conv[l]),
            fourier_mix(fx),
            multiscale_pool(px, w_pool[l], pool_scale[l]),
        ], axis=2)
        x = x + m_lat[..., 2, :] * merge_branches(y_lat, h_lat, w_branch[l], w_mgate[l], b_mgate[l], w_out[l])

        if not last:
            a_ctx = diff_head_out(diff_attend(split_qk(cq), ck_h, cv_h, lam), g_subln[l], lam_init)
            y_ctx = jnp.stack([
                a_ctx,
                short_conv(csx, csb, csc, w_conv[l]),
                fourier_mix(cfx),
                multiscale_pool(cpx, w_pool[l], pool_scale[l]),
            ], axis=2)
            ctx = ctx + m_ctx[2] * merge_branches(y_ctx, h_ctx, w_branch[l], w_mgate[l], b_mgate[l], w_out[l])

        h2 = modulate(rmsnorm(x, g_norm2[l]), m_lat[..., 3, :], m_lat[..., 4, :])
        x = x + m_lat[..., 5, :] * moe_ffn(h2, w_router[l], b_router[l], w_e_gate[l], b_e_gate[l],
                                           w_e_up[l], b_e_up[l], w_e_down[l], b_e_down[l])
        if not last:
            h2c = modulate(rmsnorm(ctx, g_norm2[l]), m_ctx[3], m_ctx[4])
            ctx = ctx + m_ctx[5] * moe_ffn(h2c, w_router[l], b_router[l], w_e_gate[l], b_e_gate[l],
                                           w_e_up[l], b_e_up[l], w_e_down[l], b_e_down[l])

    return rmsnorm(x, g_final)
```

```python
import os
import math
from contextlib import ExitStack
from collections import defaultdict
import numpy as np
import concourse.bass as bass
import concourse.mybir as mybir
from concourse.bass_utils import run_bass_kernel_spmd

F32, BF16 = mybir.dt.float32, mybir.dt.bfloat16
AF = mybir.ActivationFunctionType
ALU = mybir.AluOpType
AX = mybir.AxisListType

D = 1024; S = 8192; CTX = 256; T = S + CTX; L = 2; NE = 32; EPS = 1e-6
GRID_W = 64
LIMIT = 7.0; ALPHA = 1.702
POOL_WINDOWS = (2, 4, 8, 16)


class Buf:
    __slots__ = ("w", "r")

    def __init__(self):
        self.w = {}
        self.r = {}


class Cx:
    def __init__(self, nc, stack):
        self.nc = nc
        self.engs = {"pe": nc.tensor, "dve": nc.vector, "act": nc.scalar, "pool": nc.gpsimd, "sp": nc.sync}
        self.sems = {}
        self.cnt = {}
        for k in self.engs:
            self.sems[k] = stack.enter_context(nc.semaphore("s_" + k))
            self.cnt[k] = 0
        self.dkeys = {"sp": [], "pool": []}
        for q, n in (("sp", 16), ("pool", 1)):
            for i in range(n):
                key = "d%s%d" % (q, i)
                self.sems[key] = stack.enter_context(nc.semaphore("s_" + key))
                self.cnt[key] = 0
                self.dkeys[q].append(key)
        self.rr = {"sp": 0, "pool": 0}
        self.seen = {k: {} for k in self.engs}
        self.hist = {k: [] for k in ("pe", "dve", "act", "pool")}
        self.pending = {k: [] for k in self.engs}
        self.B = defaultdict(Buf)

    def _wait(self, E, key, c):
        if key == E:
            if E in ("pe", "sp") or c < self.cnt[E]:
                return
        if self.seen[E].get(key, 0) >= c:
            return
        self.engs[E].wait_ge(self.sems[key], c)
        self.seen[E][key] = c
        if key in self.hist:
            h = self.hist[key]
            lo, hi = 0, len(h)
            while lo < hi:
                mid = (lo + hi) // 2
                if h[mid][0] < c:
                    lo = mid + 1
                else:
                    hi = mid
            if lo > 0:
                for k2, c2 in h[lo - 1][1].items():
                    if k2 != E and self.seen[E].get(k2, 0) < c2:
                        self.seen[E][k2] = c2
        if E in self.hist:
            self.hist[E].append((self.cnt[E], dict(self.seen[E])))

    def _deps(self, E, reads, writes):
        for b in reads:
            for k, c in list(b.w.items()):
                self._wait(E, k, c)
        for b in writes:
            for k, c in list(b.w.items()):
                self._wait(E, k, c)
            for k, c in list(b.r.items()):
                self._wait(E, k, c)

    def bufs(self, names):
        return [self.B[n] for n in names]

    def op(self, E, fn, r=(), w=(), signal=True):
        reads = self.bufs(r); writes = self.bufs(w)
        self._deps(E, reads, writes)
        inst = fn()
        self.pending[E].append((reads, writes))
        if signal:
            self.cnt[E] += 1
            c = self.cnt[E]
            inst.then_inc(self.sems[E], 1)
            for rs, ws in self.pending[E]:
                for b in rs:
                    b.r[E] = c
                for b in ws:
                    b.w = {E: c}
                    b.r = {}
            self.pending[E] = []
        return inst

    def dma(self, Q, out, in_, r=(), w=(), wadd=()):
        reads = self.bufs(r); writes = self.bufs(w); wadds = self.bufs(wadd)
        keys = self.dkeys[Q]
        key = keys[self.rr[Q] % len(keys)]
        self.rr[Q] += 1
        if self.cnt[key] > 0:
            self._wait(Q, key, self.cnt[key])
        self._deps(Q, reads, writes)
        for b in wadds:
            for k, c0 in list(b.r.items()):
                self._wait(Q, k, c0)
        inst = self.engs[Q].dma_start(out=out, in_=in_)
        self.cnt[key] += 16
        c = self.cnt[key]
        inst.then_inc(self.sems[key], 16)
        for b in reads:
            b.r[key] = c
        for b in writes:
            b.w = {key: c}
            b.r = {}
        for b in wadds:
            b.w[key] = c
        return inst

    def barrier(self):
        for E in self.engs:
            assert not self.pending[E]
        for E in self.engs:
            for key, c in self.cnt.items():
                if c > 0:
                    self._wait(E, key, c)

    def finish(self):
        for q in ("sp", "pool"):
            for key in self.dkeys[q]:
                if self.cnt[key] > 0:
                    self.nc.sync.wait_ge(self.sems[key], self.cnt[key])


def _rope_tables():
    half = 32
    inv = (10000.0 ** (-np.arange(0, half, 2, dtype=np.float32) / half)).astype(np.float32)
    s = np.arange(S)
    row = (s // GRID_W).astype(np.float32); col = (s % GRID_W).astype(np.float32)
    ar = row[:, None] * inv; ac = col[:, None] * inv
    ang = np.concatenate([ar, ar, ac, ac], axis=-1).astype(np.float32)
    cos = np.cos(ang).astype(np.float32); sin = np.sin(ang).astype(np.float32)
    sign = np.where((np.arange(64) // 16) % 2 == 0, -1.0, 1.0).astype(np.float32)
    sin = sin * sign[None, :]
    cosT = np.ones((128, T), np.float32); sinT = np.zeros((128, T), np.float32)
    cosT[:, CTX:] = np.concatenate([cos.T, cos.T], axis=0)
    sinT[:, CTX:] = np.concatenate([sin.T, sin.T], axis=0)
    return cosT, sinT


def _rot_perm():
    d = np.arange(64)
    return np.where((d // 16) % 2 == 0, d + 16, d - 16)


def _dft_tables():
    n1 = np.arange(128)[:, None].astype(np.float64); k1 = np.arange(128)[None, :].astype(np.float64)
    a = 2 * np.pi * n1 * k1 / 128.0
    cs128 = np.concatenate([np.cos(a), np.sin(a)], axis=1).astype(np.float32)
    ccs = cs128.copy()
    msc = np.concatenate([-np.sin(a), np.cos(a)], axis=1).astype(np.float32)
    n2 = np.arange(64)[:, None, None].astype(np.float64)
    kk1 = np.arange(128)[None, :, None].astype(np.float64)
    k2 = np.arange(64)[None, None, :].astype(np.float64)
    ph = 2 * np.pi * (n2 * kk1 / 8192.0 + n2 * k2 / 64.0)
    g = np.stack([np.cos(ph), -np.sin(ph)], axis=2) / 1024.0
    g = g.reshape(64, 128 * 2 * 64).astype(np.float32)
    t = np.arange(256)[:, None].astype(np.float64); kp = np.arange(256)[None, :].astype(np.float64)
    b = 2 * np.pi * t * kp / 256.0
    cs256 = np.concatenate([np.cos(b), np.sin(b)], axis=1).astype(np.float32)
    cs256 = cs256.reshape(2, 128, 512).transpose(1, 0, 2).reshape(128, 1024).copy()
    return cs128, ccs, msc, g, cs256


def _invcnt_tables():
    out = np.zeros((4, T), np.float32)
    for gi, wd in enumerate(POOL_WINDOWS):
        for (a, n) in ((0, CTX), (CTX, S)):
            t = np.arange(n)
            lo = np.clip(t - wd // 2, 0, n); hi = np.clip(t + wd // 2, 0, n)
            out[gi, a:a + n] = 1.0 / (hi - lo).astype(np.float32)
    return out


def build_program(debug=False, stop_after=None, layers=L, moe=True):
    nc = bass.Bass("TRN2", target_bir_lowering=False)
    dbg_kind = "ExternalOutput" if debug else "Internal"

    def din(name, shape, dt=F32):
        return nc.dram_tensor(name, list(shape), dt, kind="ExternalInput").ap()

    def dscr(name, shape, dt):
        return nc.dram_tensor(name, list(shape), dt, kind=dbg_kind).ap()

    x_in = din("x", [S, D]); ctx_in = din("ctx", [CTX, D]); cvec = din("cvec", [128, 16])
    w_ada = din("w_ada", [L, D, 6 * D]); b_adaT = din("b_adaT", [L, 128, 48])
    g1T = din("g1T", [L, 128, 8]); g2T = din("g2T", [L, 128, 8]); gfT = din("gfT", [128, 8])
    w_in = din("w_in", [L, D, 4096]); w_in_rot = din("w_in_rot", [L, D, 1024])
    lam_rep = din("lam_rep", [L, 128, 256]); gsubT = din("gsubT", [L, 128, 1])
    w_convT = din("w_convT", [L, 128, 12]); w_pool = din("w_pool", [L, 4, 128, 128])
    pool_scaleT = din("pool_scaleT", [L, 128, 4])
    w_branch = din("w_branch", [L, 4, 512, D]); w_mgate = din("w_mgate", [L, D, 4 * D])
    b_mgateT = din("b_mgateT", [L, 128, 32]); w_out = din("w_out", [L, D, D])
    w_router = din("w_router", [L, D, NE]); b_router_rep = din("b_router_rep", [L, 128, NE])
    ned = NE if moe else 1
    w_eg = din("w_e_gate", [L, ned, D, D]); w_eu = din("w_e_up", [L, ned, D, D]); w_ed = din("w_e_down", [L, ned, D, D])
    b_egT = din("b_egT", [L, 128, NE * 8]); b_euT = din("b_euT", [L, 128, NE * 8]); b_ed = din("b_e_down", [L, NE, D])
    rope_cos = din("rope_cos", [128, T]); rope_sin = din("rope_sin", [128, T])
    ident_in = din("ident", [128, 128])
    cs128_in = din("cs128", [128, 256]); ccs_in = din("ccs", [128, 256]); msc_in = din("msc", [128, 256])
    gtab_in = din("gtab", [64, 128 * 2 * 64]); cs256_in = din("cs256", [128, 1024])
    invcnt_in = din("invcnt", [4, T])
    out_ap = nc.dram_tensor("out", [S, D], F32, kind="ExternalOutput").ap()

    XT = dscr("XT", [D, T], F32)
    HT = dscr("HT", [D, T], BF16)
    QT = dscr("QT", [512, T], BF16); KT = dscr("KT", [512, T], BF16); VV = dscr("VV", [T, 512], BF16)
    SXT = dscr("SXT", [512, T], BF16); SBT = dscr("SBT", [512, T], BF16); SCT = dscr("SCT", [512, T], BF16)
    FX = dscr("FX", [T, 512], BF16); PXT = dscr("PXT", [512, T], BF16)
    YY = dscr("YY", [4, 512, T], BF16)
    H2T = dscr("H2T", [D, T], BF16)
    COMBT = dscr("COMBT", [NE, T], F32)

    XTv = XT.rearrange("(k p) t -> p k t", p=128)
    HTv = HT.rearrange("(k p) t -> p k t", p=128)
    H2Tv = H2T.rearrange("(k p) t -> p k t", p=128)

    stack = ExitStack()
    with stack:
        cx = Cx(nc, stack)
        B = cx.B

        uid = [0]

        def G(name, t0, n):
            return ["%s_%d" % (name, g) for g in range(t0 // 256, (t0 + n + 255) // 256)]

        def GA(name):
            return ["%s_%d" % (name, g) for g in range(T // 256)]

        def sb(st, name, shape, dt):
            uid[0] += 1
            return st.enter_context(nc.sbuf_tensor("%s_u%d" % (name, uid[0]), list(shape), dt))

        def ps(st, name, dt=F32, cols=512):
            uid[0] += 1
            return st.enter_context(nc.psum_tensor("%s_u%d" % (name, uid[0]), [128, cols], dt))

        ones_bf = sb(stack, "ones_bf", [128, 128], BF16)
        ident = sb(stack, "ident", [128, 128], F32)
        mods = sb(stack, "mods", [128, 48, 2], F32)
        gs1 = sb(stack, "gs1", [128, 2, 8], F32); gs2 = sb(stack, "gs2", [128, 2, 8], F32)
        sh1 = sb(stack, "sh1", [128, 2, 8], F32); sh2 = sb(stack, "sh2", [128, 2, 8], F32)
        gt1 = sb(stack, "gt1", [128, 2, 8], F32); gt2 = sb(stack, "gt2", [128, 2, 8], F32)
        cx.op("dve", lambda: nc.vector.memset(ones_bf[:], 1.0), w=["ones"])
        epsc = sb(stack, "epsc", [128, 1], F32)
        cx.op("dve", lambda: nc.vector.memset(epsc[:], EPS), w=["epsc"])
        cx.dma("sp", ident[:], ident_in, w=["ident"])

        def phase_B():
            with ExitStack() as st:
                xin = [sb(st, "xin%d" % i, [128, D], F32) for i in range(2)]
                xo = [sb(st, "xo%d" % i, [128, 8, 128], F32) for i in range(2)]
                pt = [ps(st, "ptB%d" % i) for i in range(2)]
                ntile = T // 128
                for j in range(ntile):
                    src = ctx_in[j * 128:(j + 1) * 128, :] if j < 2 else x_in[(j - 2) * 128:(j - 1) * 128, :]
                    xi = xin[j % 2]; xoo = xo[j % 2]
                    cx.dma("sp", xi[:], src, w=["xin%d" % (j % 2)])
                    for hf in range(2):
                        p = pt[hf]
                        for q in range(4):
                            k = hf * 4 + q
                            cx.op("pe", lambda: nc.tensor.transpose(p[:, q * 128:(q + 1) * 128], xi[:, k * 128:(k + 1) * 128], ident[:]),
                                  r=["xin%d" % (j % 2), "ident"], w=["ptB%d" % hf], signal=(q == 3))
                        eng = "dve" if hf == 0 else "act"
                        if hf == 0:
                            cx.op("dve", lambda: nc.vector.tensor_copy(out=xoo[:, 0:4, :], in_=p[:, :].rearrange("p (q t) -> p q t", q=4)),
                                  r=["ptB0"], w=["xo%d_0" % (j % 2)])
                        else:
                            cx.op("act", lambda: nc.scalar.copy(out=xoo[:, 4:8, :], in_=p[:, :].rearrange("p (q t) -> p q t", q=4)),
                                  r=["ptB1"], w=["xo%d_1" % (j % 2)])
                    cx.dma("sp", XTv[:, :, j * 128:(j + 1) * 128], xoo[:], r=["xo%d_0" % (j % 2), "xo%d_1" % (j % 2)], w=G("XT", j * 128, 128) if j % 2 == 0 else (), wadd=() if j % 2 == 0 else G("XT", j * 128, 128))

        def phase_A(l):
            with ExitStack() as st:
                cv = sb(st, "cv", [128, 16], F32); sc = sb(st, "scv", [128, 8, 2], F32)
                sg = sb(st, "sgv", [128, 16], F32)
                wa = [sb(st, "wa%d" % i, [128, 8, 1024], F32) for i in range(2)]
                bT = sb(st, "bT", [128, 48], F32); g1 = sb(st, "g1", [128, 8], F32); g2 = sb(st, "g2", [128, 8], F32)
                pm = ps(st, "pmA")
                cx.dma("sp", cv[:], cvec, w=["cv"])
                cx.dma("sp", bT[:], b_adaT[l], w=["bT"])
                cx.dma("sp", g1[:], g1T[l], w=["g1"])
                cx.dma("sp", g2[:], g2T[l], w=["g2"])
                cx.op("act", lambda: nc.scalar.activation(out=sg[:], in_=cv[:], func=AF.Sigmoid), r=["cv"], w=["sgv"])
                cx.op("dve", lambda: nc.vector.tensor_tensor(out=sc[:, :, 0], in0=cv[:, 0:8], in1=sg[:, 0:8], op=ALU.mult), r=["cv", "sgv"], w=["scv"])
                cx.op("dve", lambda: nc.vector.tensor_tensor(out=sc[:, :, 1], in0=cv[:, 8:16], in1=sg[:, 8:16], op=ALU.mult), r=["cv", "sgv"], w=["scv"])
                wav = w_ada[l].rearrange("(k p) n -> p k n", p=128)
                for wch in range(6):
                    wt = wa[wch % 2]
                    for k in range(8):
                        cx.dma("sp", wt[:, k, :], wav[:, k, wch * 1024:(wch + 1) * 1024], w=["wa%d" % (wch % 2)] if k == 0 else (), wadd=() if k == 0 else ["wa%d" % (wch % 2)])
                    for jj in range(8):
                        j = wch * 8 + jj
                        for k in range(8):
                            cx.op("pe", lambda: nc.tensor.matmul(pm[:, 2 * j:2 * j + 2], lhsT=wt[:, k, jj * 128:(jj + 1) * 128], rhs=sc[:, k, :],
                                                                 start=(k == 0), stop=(k == 7)),
                                  r=["wa%d" % (wch % 2), "scv"], w=["pmA"], signal=(k == 7))
                cx.op("dve", lambda: nc.vector.tensor_copy(out=mods[:].rearrange("p j c -> p (j c)"), in_=pm[:, 0:96]), r=["pmA"], w=["mods"])
                for c in range(2):
                    cx.op("dve", lambda: nc.vector.tensor_tensor(out=mods[:, :, c], in0=mods[:, :, c], in1=bT[:], op=ALU.add), r=["mods", "bT"], w=["mods"])
                for c in range(2):
                    cx.op("dve", lambda: nc.vector.scalar_tensor_tensor(out=gs1[:, c, :], in0=mods[:, 8:16, c], scalar=1.0, in1=g1[:], op0=ALU.add, op1=ALU.mult),
                          r=["mods", "g1"], w=["modv"])
                    cx.op("dve", lambda: nc.vector.scalar_tensor_tensor(out=gs2[:, c, :], in0=mods[:, 32:40, c], scalar=1.0, in1=g2[:], op0=ALU.add, op1=ALU.mult),
                          r=["mods", "g2"], w=["modv"])
                    cx.op("dve", lambda: nc.vector.tensor_copy(out=sh1[:, c, :], in_=mods[:, 0:8, c]), r=["mods"], w=["modv"])
                    cx.op("dve", lambda: nc.vector.tensor_copy(out=gt1[:, c, :], in_=mods[:, 16:24, c]), r=["mods"], w=["modv"])
                    cx.op("dve", lambda: nc.vector.tensor_copy(out=sh2[:, c, :], in_=mods[:, 24:32, c]), r=["mods"], w=["modv"])
                    cx.op("dve", lambda: nc.vector.tensor_copy(out=gt2[:, c, :], in_=mods[:, 40:48, c]), r=["mods"], w=["modv"])

        def norm_block(st_tiles, xt, n, gs, sh, mi, hT, h32=None, tag=""):
            sq, pss, R, tmp = st_tiles
            nstop = int(os.environ.get("NSTOP", "9"))
            if nstop < 1:
                return
            for k in range(8):
                cx.op("dve", lambda: nc.vector.tensor_tensor(out=sq[:, k, :n], in0=xt[:, k, :n], in1=xt[:, k, :n], op=ALU.mult), r=["xt" + tag], w=["sq"])
            if nstop < 2:
                return
            for k in range(8):
                cx.op("pe", lambda: nc.tensor.matmul(pss[:, :n], lhsT=ones_bf[:], rhs=sq[:, k, :n], start=(k == 0), stop=(k == 7)),
                      r=["sq", "ones"], w=["pss"], signal=(k == 7))
            if nstop < 3:
                return
            cx.op("act", lambda: nc.scalar.activation(out=R[:, :n], in_=pss[:, :n], func=AF.Sqrt, scale=1.0 / D, bias=epsc[:, 0:1]), r=["pss", "epsc"], w=["R"])
            if nstop < 4:
                return
            cx.op("dve", lambda: nc.vector.reciprocal(out=R[:, :n], in_=R[:, :n]), r=["R"], w=["R"])
            if nstop < 5:
                return
            for k in range(8):
                cx.op("dve", lambda: nc.vector.scalar_tensor_tensor(out=tmp[:, k, :n], in0=xt[:, k, :n], scalar=gs[:, mi, k:k + 1], in1=R[:, :n], op0=ALU.mult, op1=ALU.mult),
                      r=["xt" + tag, "R", "modv"], w=["ntmp"])
                if nstop < 6:
                    continue
                if h32 is not None:
                    cx.op("act", lambda: nc.scalar.activation(out=h32[:, k, :n], in_=tmp[:, k, :n], func=AF.Identity, bias=sh[:, mi, k:k + 1], scale=1.0),
                          r=["ntmp", "modv"], w=["h32"])
                    cx.op("pool", lambda: nc.gpsimd.tensor_copy(out=hT[:, k, :n], in_=h32[:, k, :n]), r=["h32"], w=["hT" + tag])
                else:
                    cx.op("act", lambda: nc.scalar.activation(out=hT[:, k, :n], in_=tmp[:, k, :n], func=AF.Identity, bias=sh[:, mi, k:k + 1], scale=1.0),
                          r=["ntmp", "modv"], w=["hT" + tag])

        def blocks512(lo):
            bl = []
            if lo == 0:
                bl.append((0, CTX))
            for i in range(S // 512):
                bl.append((CTX + i * 512, 512))
            return bl

        def phase_C(l):
            with ExitStack() as st:
                win = sb(st, "win", [128, 8, 4096], BF16); winr = sb(st, "winr", [128, 8, 1024], BF16)
                wv = w_in[l].rearrange("(k p) n -> p k n", p=128); wrv = w_in_rot[l].rearrange("(k p) n -> p k n", p=128)
                for k in range(8):
                    cx.dma("pool", win[:, k, :], wv[:, k, :], w=["win"] if k == 0 else (), wadd=() if k == 0 else ["win"])
                for k in range(8):
                    cx.dma("pool", winr[:, k, :], wrv[:, k, :], w=["winr"] if k == 0 else (), wadd=() if k == 0 else ["winr"])
                xts = [sb(st, "xtC%d" % i, [128, 8, 512], F32) for i in range(2)]
                hTs = [sb(st, "hTC%d" % i, [128, 8, 512], BF16) for i in range(2)]
                sq = sb(st, "sqC", [128, 8, 512], BF16); R = sb(st, "RC", [128, 512], F32); tmp = sb(st, "tmpC", [128, 8, 512], F32)
                cosb = [sb(st, "cosb%d" % i, [128, 512], F32) for i in range(2)]
                sinb = [sb(st, "sinb%d" % i, [128, 512], F32) for i in range(2)]
                t1 = [sb(st, "t1C%d" % i, [128, 512], F32) for i in range(2)]
                t2 = [sb(st, "t2C%d" % i, [128, 512], F32) for i in range(2)]
                stg = [sb(st, "stgC%d" % i, [128, 4, 512], BF16) for i in range(3)]
                stgt = [sb(st, "stgT%d" % i, [128, 512], BF16) for i in range(3)]
                pss = ps(st, "pssC")
                pp = [ps(st, "ppC%d" % i) for i in range(6)]
                bl = blocks512(0)
                stg_i = 0; stgt_i = 0; pp_i = 0
                cstop = int(os.environ.get("CSTOP", "9"))
                nblk = int(os.environ.get("CBLK", "99"))
                for bi, (t0, n) in enumerate(bl):
                    if bi >= nblk:
                        break
                    mi = 1 if t0 < CTX else 0
                    xt = xts[bi % 2]; hT = hTs[bi % 2]; tg = "C%d" % (bi % 2)
                    cx.dma("sp", xt[:, :, :n], XTv[:, :, t0:t0 + n], r=G("XT", t0, n), w=["xt" + tg])
                    cx.dma("sp", cosb[bi % 2][:, :n], rope_cos[:, t0:t0 + n], w=["cosb%d" % (bi % 2)])
                    cx.dma("sp", sinb[bi % 2][:, :n], rope_sin[:, t0:t0 + n], w=["sinb%d" % (bi % 2)])
                    norm_block((sq, pss, R, tmp), xt, n, gs1, sh1, mi, hT, tag=tg)
                    cx.dma("sp", HTv[:, :, t0:t0 + n], hT[:, :, :n], r=["hT" + tg], w=G("HT", t0, n))

                    def proj(wt, wname, c0, p, pname):
                        for k in range(8):
                            cx.op("pe", lambda: nc.tensor.matmul(p[:, :n], lhsT=wt[:, k, c0:c0 + 128], rhs=hT[:, k, :n], start=(k == 0), stop=(k == 7)),
                                  r=[wname, "hT" + tg], w=[pname], signal=(k == 7))
                    if cstop < 1:
                        continue
                    for which, dst, dname in ((0, QT, "QT"), (1, KT, "KT")):
                        sg_ = stg[stg_i % 3]; sname = "stgC%d" % (stg_i % 3); stg_i += 1
                        for h in range(4):
                            pa = pp[pp_i % 6]; pan = "ppC%d" % (pp_i % 6); pp_i += 1
                            pb = pp[pp_i % 6]; pbn = "ppC%d" % (pp_i % 6); pp_i += 1
                            proj(win, "win", which * 512 + h * 128, pa, pan)
                            proj(winr, "winr", which * 512 + h * 128, pb, pbn)
                            a1 = t1[h % 2]; a2 = t2[h % 2]
                            cx.op("dve", lambda: nc.vector.tensor_tensor(out=a1[:, :n], in0=pa[:, :n], in1=cosb[bi % 2][:, :n], op=ALU.mult),
                                  r=[pan, "cosb%d" % (bi % 2)], w=["t1C%d" % (h % 2)])
                            cx.op("dve", lambda: nc.vector.tensor_tensor(out=a2[:, :n], in0=pb[:, :n], in1=sinb[bi % 2][:, :n], op=ALU.mult),
                                  r=[pbn, "sinb%d" % (bi % 2)], w=["t2C%d" % (h % 2)])
                            cx.op("pool", lambda: nc.gpsimd.tensor_tensor(out=sg_[:, h, :n], in0=a1[:, :n], in1=a2[:, :n], op=ALU.add),
                                  r=["t1C%d" % (h % 2), "t2C%d" % (h % 2)], w=[sname])
                        cx.dma("sp", dst.rearrange("(h p) t -> p h t", p=128)[:, :, t0:t0 + n], sg_[:, :, :n], r=[sname], w=G(dname, t0, n))
                    if cstop < 2:
                        continue
                    for c0, dst, dname in ((1536, SXT, "SXT"), (2048, SBT, "SBT"), (2560, SCT, "SCT"), (3584, PXT, "PXT")):
                        sg_ = stg[stg_i % 3]; sname = "stgC%d" % (stg_i % 3); stg_i += 1
                        for h in range(4):
                            pa = pp[pp_i % 6]; pan = "ppC%d" % (pp_i % 6); pp_i += 1
                            proj(win, "win", c0 + h * 128, pa, pan)
                            if h % 2 == 0:
                                cx.op("act", lambda: nc.scalar.copy(out=sg_[:, h, :n], in_=pa[:, :n]), r=[pan], w=[sname])
                            else:
                                cx.op("dve", lambda: nc.vector.tensor_copy(out=sg_[:, h, :n], in_=pa[:, :n]), r=[pan], w=[sname])
                        cx.dma("sp", dst.rearrange("(h p) t -> p h t", p=128)[:, :, t0:t0 + n], sg_[:, :, :n], r=[sname], w=G(dname, t0, n))
                    if cstop < 3:
                        continue
                    for c0, dst, dname in ((1024, VV, "VV"), (3072, FX, "FX")):
                        for j in range(n // 128):
                            pa = pp[pp_i % 6]; pan = "ppC%d" % (pp_i % 6); pp_i += 1
                            so = stgt[stgt_i % 3]; son = "stgT%d" % (stgt_i % 3); stgt_i += 1
                            for k in range(8):
                                cx.op("pe", lambda: nc.tensor.matmul(pa[:, :], lhsT=hT[:, k, j * 128:(j + 1) * 128], rhs=win[:, k, c0:c0 + 512], start=(k == 0), stop=(k == 7)),
                                      r=["win", "hT" + tg], w=[pan], signal=(k == 7))
                            if j % 2 == 0:
                                cx.op("act", lambda: nc.scalar.copy(out=so[:], in_=pa[:, :]), r=[pan], w=[son])
                            else:
                                cx.op("dve", lambda: nc.vector.tensor_copy(out=so[:], in_=pa[:, :]), r=[pan], w=[son])
                            cx.dma("sp", dst[t0 + j * 128:t0 + (j + 1) * 128, :], so[:], r=[son], w=G(dname, t0, n) if j == 0 else (), wadd=() if j == 0 else G(dname, t0, n))

        def phase_D(l):
            lam_init = 0.8 - 0.6 * math.exp(-0.3 * l)
            with ExitStack() as st:
                lamt = sb(st, "lamt", [128, 256], F32); lp = sb(st, "lp", [128, 128], F32); l2 = sb(st, "l2", [128, 2], F32)
                nlam = sb(st, "nlam", [128, 1], F32); gsub = sb(st, "gsub", [128, 1], F32)
                cx.dma("sp", lamt[:], lam_rep[l], w=["lamt"])
                cx.dma("sp", gsub[:], gsubT[l], w=["gsub"])
                cx.op("dve", lambda: nc.vector.tensor_tensor(out=lp[:, 0:64], in0=lamt[:, 0:64], in1=lamt[:, 64:128], op=ALU.mult), r=["lamt"], w=["lp"])
                cx.op("dve", lambda: nc.vector.tensor_tensor(out=lp[:, 64:128], in0=lamt[:, 128:192], in1=lamt[:, 192:256], op=ALU.mult), r=["lamt"], w=["lp"])
                cx.op("dve", lambda: nc.vector.reduce_sum(out=l2[:, :], in_=lp[:].rearrange("p (a b) -> p a b", a=2), axis=AX.X), r=["lp"], w=["l2"])
                cx.op("act", lambda: nc.scalar.activation(out=l2[:], in_=l2[:], func=AF.Exp), r=["l2"], w=["l2"])
                cx.op("dve", lambda: nc.vector.scalar_tensor_tensor(out=nlam[:], in0=l2[:, 1:2], scalar=-lam_init, in1=l2[:, 0:1], op0=ALU.add, op1=ALU.subtract),
                      r=["l2"], w=["nlam"])
                cx.op("dve", lambda: nc.vector.tensor_scalar(out=gsub[:], in0=gsub[:], scalar1=(1.0 - lam_init), scalar2=None, op0=ALU.mult), r=["gsub"], w=["gsub"])

                kts = [sb(st, "ktD%d" % i, [128, T], BF16) for i in range(2)]
                vts = [sb(st, "vtD%d" % i, [128, T // 128, 128], BF16) for i in range(2)]
                qts = [sb(st, "qtD%d" % i, [128, 512], BF16) for i in range(2)]
                ets = [sb(st, "etD%d" % i, [128, 2, 512], BF16) for i in range(3)]
                zacc = sb(st, "zaccD", [128, 512], F32)
                ones_f = sb(st, "onesfD", [128, 128], F32)
                cx.op("dve", lambda: nc.vector.memset(ones_f[:], 1.0), w=["onesf"])
                r0 = sb(st, "r0D", [128, 512], F32); r1 = sb(st, "r1D", [128, 512], F32)
                o0 = sb(st, "o0D", [128, 512], F32); o1 = sb(st, "o1D", [128, 512], F32)
                av = sb(st, "avD", [128, 512], F32); a2 = sb(st, "a2D", [128, 512], BF16); rr = sb(st, "rrD", [128, 512], F32)
                yo = [sb(st, "yoD%d" % i, [128, 512], BF16) for i in range(2)]
                pS = [ps(st, "pSD%d" % i, cols=1024) for i in range(2)]
                pO = [ps(st, "pOD%d" % i) for i in range(2)]
                pZ = [ps(st, "pZD%d" % i) for i in range(2)]
                VVv = VV.rearrange("(j p) c -> p j c", p=128)
                bl = blocks512(l)
                nkt = T // 128
                et_i = 0; ps_i = 0; qi = 0
                for h in range(4):
                    kt = kts[h % 2]; vt = vts[h % 2]
                    cx.dma("sp", kt[:], KT[h * 128:(h + 1) * 128, :], r=GA("KT"), w=["ktD%d" % (h % 2)])
                    for jj in range(0, nkt, 11):
                        cx.dma("sp", vt[:, jj:jj + 11, :], VVv[:, jj:jj + 11, h * 128:(h + 1) * 128], r=GA("VV"), w=["vtD%d" % (h % 2)] if jj == 0 else (), wadd=() if jj == 0 else ["vtD%d" % (h % 2)])
                    for (t0, n) in bl:
                        isctx = t0 < CTX
                        ktiles = range(2) if isctx else range(nkt)
                        qt = qts[qi % 2]; qn = "qtD%d" % (qi % 2); qi += 1
                        cx.dma("sp", qt[:, :n], QT[h * 128:(h + 1) * 128, t0:t0 + n], r=G("QT", t0, n), w=[qn])
                        nk = len(ktiles)
                        kl = list(ktiles)
                        slots = {}

                        def emit_qk(idx):
                            nonlocal ps_i, et_i
                            j = kl[idx]
                            pS_ = pS[ps_i % 2]; psn = "pSD%d" % (ps_i % 2); ps_i += 1
                            et = ets[et_i % 3]; etn = "etD%d" % (et_i % 3); et_i += 1
                            for i in range(2):
                                cx.op("pe", lambda: nc.tensor.matmul(pS_[:, i * 512:i * 512 + n], lhsT=kt[64 * i:64 * i + 64, j * 128:(j + 1) * 128], rhs=qt[64 * i:64 * i + 64, :n], start=True, stop=True),
                                      r=["ktD%d" % (h % 2), qn], w=[psn, etn], signal=(i == 1))
                            cx.op("act", lambda: nc.scalar.activation(out=et[:, :, :n], in_=pS_[:, :].rearrange("p (i c) -> p i c", i=2)[:, :, :n], func=AF.Exp, scale=0.125), r=[psn], w=[etn])
                            slots[idx] = (et, etn)

                        def emit_pv(idx):
                            j = kl[idx]
                            et, etn = slots.pop(idx)
                            cx.op("pe", lambda: nc.tensor.matmul(pO[0][:, :n], lhsT=vt[:, j, :], rhs=et[:, 0, :n], start=(idx == 0), stop=(idx == nk - 1)),
                                  r=["vtD%d" % (h % 2), etn], w=["pOD0"], signal=False)
                            cx.op("pe", lambda: nc.tensor.matmul(pZ[0][:, :n], lhsT=ones_bf[:], rhs=et[:, 0, :n], start=(idx == 0), stop=(idx == nk - 1)),
                                  r=["ones", etn], w=["pZD0"], signal=False)
                            cx.op("pe", lambda: nc.tensor.matmul(pO[1][:, :n], lhsT=vt[:, j, :], rhs=et[:, 1, :n], start=(idx == 0), stop=(idx == nk - 1)),
                                  r=["vtD%d" % (h % 2), etn], w=["pOD1"], signal=True)
                            if idx == 0:
                                cx.op("dve", lambda: nc.vector.tensor_copy(out=zacc[:, :n], in_=et[:, 1, :n]), r=[etn], w=["zaccD"])
                            else:
                                cx.op("dve", lambda: nc.vector.tensor_tensor(out=zacc[:, :n], in0=zacc[:, :n], in1=et[:, 1, :n], op=ALU.add), r=[etn, "zaccD"], w=["zaccD"])

                        emit_qk(0)
                        for idx in range(nk):
                            if idx + 1 < nk:
                                emit_qk(idx + 1)
                            emit_pv(idx)
                        cx.op("pe", lambda: nc.tensor.matmul(pZ[1][:, :n], lhsT=ones_f[:], rhs=zacc[:, :n], start=True, stop=True), r=["onesf", "zaccD"], w=["pZD1"])
                        cx.op("dve", lambda: nc.vector.reciprocal(out=r0[:, :n], in_=pZ[0][:, :n]), r=["pZD0"], w=["r0D"])
                        cx.op("dve", lambda: nc.vector.reciprocal(out=r1[:, :n], in_=pZ[1][:, :n]), r=["pZD1"], w=["r1D"])
                        cx.op("dve", lambda: nc.vector.tensor_tensor(out=o0[:, :n], in0=pO[0][:, :n], in1=r0[:, :n], op=ALU.mult), r=["pOD0", "r0D"], w=["o0D"])
                        cx.op("dve", lambda: nc.vector.tensor_tensor(out=o1[:, :n], in0=pO[1][:, :n], in1=r1[:, :n], op=ALU.mult), r=["pOD1", "r1D"], w=["o1D"])
                        cx.op("dve", lambda: nc.vector.scalar_tensor_tensor(out=av[:, :n], in0=o1[:, :n], scalar=nlam[:, 0:1], in1=o0[:, :n], op0=ALU.mult, op1=ALU.add),
                              r=["o0D", "o1D", "nlam"], w=["avD"])
                        cx.op("dve", lambda: nc.vector.tensor_tensor(out=a2[:, :n], in0=av[:, :n], in1=av[:, :n], op=ALU.mult), r=["avD"], w=["a2D"])
                        pn = pS[ps_i % 2]; pnn = "pSD%d" % (ps_i % 2); ps_i += 1
                        cx.op("pe", lambda: nc.tensor.matmul(pn[:, :n], lhsT=ones_bf[:], rhs=a2[:, :n], start=True, stop=True), r=["ones", "a2D"], w=[pnn])
                        cx.op("act", lambda: nc.scalar.activation(out=rr[:, :n], in_=pn[:, :n], func=AF.Sqrt, scale=1.0 / 128, bias=epsc[:, 0:1]), r=[pnn, "epsc"], w=["rrD"])
                        cx.op("dve", lambda: nc.vector.reciprocal(out=rr[:, :n], in_=rr[:, :n]), r=["rrD"], w=["rrD"])
                        y = yo[qi % 2]; yn = "yoD%d" % (qi % 2)
                        cx.op("dve", lambda: nc.vector.scalar_tensor_tensor(out=y[:, :n], in0=av[:, :n], scalar=gsub[:, 0:1], in1=rr[:, :n], op0=ALU.mult, op1=ALU.mult),
                              r=["avD", "rrD", "gsub"], w=[yn])
                        cx.dma("sp", YY[0, h * 128:(h + 1) * 128, t0:t0 + n], y[:, :n], r=[yn], w=G("YY0h%d" % h, t0, n))

        def segs(l):
            return ([(0, CTX)] if l == 0 else []) + [(CTX, S)]

        def phase_E_conv(l):
            with ExitStack() as st:
                wc = sb(st, "wcE", [128, 12], F32)
                cx.dma("sp", wc[:], w_convT[l], w=["wcE"])
                sx = sb(st, "sxE", [128, T], BF16); sbb = sb(st, "sbE", [128, T], BF16); scc = sb(st, "scE", [128, T], BF16)
                u = sb(st, "uE", [128, T], F32); y = sb(st, "yE", [128, T], F32); yb = sb(st, "ybE", [128, T], BF16)
                lo = 0 if l == 0 else CTX
                for cc in range(4):
                    rows = slice(cc * 128, (cc + 1) * 128)
                    allb = list(range(17))
                    cx.dma("sp", sx[:, lo:], SXT[rows, lo:], r=GA("SXT"), w=["sxE"])
                    cx.dma("sp", sbb[:, lo:], SBT[rows, lo:], r=GA("SBT"), w=["sbE"])
                    cx.dma("sp", scc[:, lo:], SCT[rows, lo:], r=GA("SCT"), w=["scE"])
                    cx.op("dve", lambda: nc.vector.tensor_tensor(out=u[:, lo:], in0=scc[:, lo:], in1=sx[:, lo:], op=ALU.mult), r=["sxE", "scE"], w=["uE"])
                    for (a, n) in segs(l):
                        b = a + n
                        cx.op("dve", lambda: nc.vector.tensor_scalar(out=y[:, a:b], in0=u[:, a:b], scalar1=wc[:, cc * 3 + 1:cc * 3 + 2], scalar2=None, op0=ALU.mult),
                              r=["uE", "wcE"], w=["yE"])
                        cx.op("dve", lambda: nc.vector.scalar_tensor_tensor(out=y[:, a + 1:b], in0=u[:, a:b - 1], scalar=wc[:, cc * 3:cc * 3 + 1], in1=y[:, a + 1:b], op0=ALU.mult, op1=ALU.add),
                              r=["uE", "wcE", "yE"], w=["yE"])
                        cx.op("dve", lambda: nc.vector.scalar_tensor_tensor(out=y[:, a:b - 1], in0=u[:, a + 1:b], scalar=wc[:, cc * 3 + 2:cc * 3 + 3], in1=y[:, a:b - 1], op0=ALU.mult, op1=ALU.add),
                              r=["uE", "wcE", "yE"], w=["yE"])
                    cx.op("pool", lambda: nc.gpsimd.tensor_tensor(out=yb[:, lo:], in0=y[:, lo:], in1=sbb[:, lo:], op=ALU.mult), r=["yE", "sbE"], w=["ybE"])
                    cx.dma("sp", YY[1, rows, lo:], yb[:, lo:], r=["ybE"], w=["YY1_%d" % cc])

        def phase_E_pool(l):
            PAD = 16
            with ExitStack() as st:
                wp = sb(st, "wpE", [128, 4, 128], BF16); psc = sb(st, "pscE", [128, 4], F32)
                cx.dma("pool", wp[:], w_pool[l].rearrange("g c d -> c g d"), w=["wpE"])
                cx.dma("sp", psc[:], pool_scaleT[l], w=["pscE"])
                xb = sb(st, "xbP", [128, T], BF16)
                xp = sb(st, "xpP", [128, S + 2 * PAD], F32); wa = sb(st, "waP", [128, S + 2 * PAD], F32); wb = sb(st, "wbP", [128, S + 2 * PAD], F32)
                ic = sb(st, "icP", [128, T], F32); pl = sb(st, "plP", [128, T], BF16)
                yo = [sb(st, "yoP%d" % i, [128, 512], BF16) for i in range(2)]
                pq = [ps(st, "pqP%d" % i) for i in range(2)]
                lo = 0 if l == 0 else CTX
                cnt = 0
                for g in range(4):
                    rows = slice(g * 128, (g + 1) * 128)
                    cx.dma("sp", xb[:, lo:], PXT[rows, lo:], r=GA("PXT"), w=["xbP"])
                    cx.dma("sp", ic[:, lo:], invcnt_in[g:g + 1, lo:].partition_broadcast(128), w=["icP"])
                    for (a, n) in segs(l):
                        cx.op("pool", lambda: nc.gpsimd.memset(xp[:, 0:PAD], 0.0), w=["xpP"])
                        cx.op("pool", lambda: nc.gpsimd.memset(xp[:, PAD + n:PAD + n + PAD], 0.0), w=["xpP"])
                        cx.op("dve", lambda: nc.vector.tensor_copy(out=xp[:, PAD:PAD + n], in_=xb[:, a:a + n]), r=["xbP"], w=["xpP"])
                        tot = n + 2 * PAD
                        cx.op("dve", lambda: nc.vector.tensor_tensor(out=wa[:, 1:tot], in0=xp[:, 0:tot - 1], in1=xp[:, 1:tot], op=ALU.add), r=["xpP"], w=["waP"])
                        cur, curn, oth, othn = wa, "waP", wb, "wbP"
                        lo_v = 1; hi_v = tot
                        for step in range(g):
                            sft = 1 << step
                            nlo = lo_v + sft; nhi = hi_v - sft
                            cx.op("dve", lambda: nc.vector.tensor_tensor(out=oth[:, nlo:nhi], in0=cur[:, nlo - sft:nhi - sft], in1=cur[:, nlo + sft:nhi + sft], op=ALU.add),
                                  r=[curn], w=[othn])
                            cur, curn, oth, othn = oth, othn, cur, curn
                            lo_v, hi_v = nlo, nhi
                        cx.op("dve", lambda: nc.vector.tensor_tensor(out=oth[:, PAD:PAD + n], in0=cur[:, PAD:PAD + n], in1=ic[:, a:a + n], op=ALU.mult), r=[curn, "icP"], w=[othn])
                        cx.op("dve", lambda: nc.vector.tensor_tensor(out=pl[:, a:a + n], in0=oth[:, PAD:PAD + n], in1=xp[:, PAD:PAD + n], op=ALU.subtract), r=[othn, "xpP"], w=["plP"])
                    for (t0, n) in blocks512(l):
                        p = pq[cnt % 2]; pn = "pqP%d" % (cnt % 2); y = yo[cnt % 2]; yn = "yoP%d" % (cnt % 2); cnt += 1
                        cx.op("pe", lambda: nc.tensor.matmul(p[:, :n], lhsT=wp[:, g, :], rhs=pl[:, t0:t0 + n], start=True, stop=True), r=["wpE", "plP"], w=[pn])
                        cx.op("act", lambda: nc.scalar.activation(out=y[:, :n], in_=p[:, :n], func=AF.Identity, scale=psc[:, g:g + 1]), r=[pn, "pscE"], w=[yn])
                        cx.dma("sp", YY[3, rows, t0:t0 + n], y[:, :n], r=[yn], w=G("YY3g%d" % g, t0, n))

        def phase_E_fourier(l):
            with ExitStack() as st:
                cs128 = sb(st, "cs128", [128, 256], BF16); ccs = sb(st, "ccs", [128, 256], BF16); msc = sb(st, "msc", [128, 256], BF16)
                gtab = sb(st, "gtab", [64, 128 * 128], BF16)
                cx.dma("pool", cs128[:], cs128_in, w=["cs128"]); cx.dma("pool", ccs[:], ccs_in, w=["ccs"]); cx.dma("pool", msc[:], msc_in, w=["msc"])
                for q in range(4):
                    cx.dma("pool", gtab[:, q * 4096:(q + 1) * 4096], gtab_in[:, q * 4096:(q + 1) * 4096], w=["gtab"] if q == 0 else (), wadd=() if q == 0 else ["gtab"])
                gv = gtab[:].rearrange("p (k c m) -> p k c m", k=128, c=2)
                Xs = [sb(st, "XF%d" % i, [128, 64, 128], BF16) for i in range(2)]
                Xv = FX[CTX:, :].rearrange("(a b) c -> a b c", b=64)
                AT = sb(st, "ATF", [128, 128, 2, 64], BF16)
                W = sb(st, "WF", [64, 64, 256], BF16)
                YT = sb(st, "YTF", [128, S], BF16)
                pf = [ps(st, "pfF%d" % i) for i in range(4)]
                pi = 0
                for g in range(4):
                    X = Xs[g % 2]; xn = "XF%d" % (g % 2)
                    for q in range(4):
                        cx.dma("sp", X[:, q * 16:(q + 1) * 16, :], Xv[:, q * 16:(q + 1) * 16, g * 128:(g + 1) * 128], r=GA("FX"),
                               w=[xn] if q == 0 else (), wadd=() if q == 0 else [xn])
                    for n2 in range(64):
                        p = pf[pi % 4]; pn = "pfF%d" % (pi % 4); pi += 1
                        cx.op("pe", lambda: nc.tensor.matmul(p[:, 0:256], lhsT=X[:, n2, :], rhs=cs128[:], start=True, stop=True), r=[xn, "cs128"], w=[pn])
                        src = p[:, 0:256].rearrange("p (c k) -> p c k", c=2)
                        dstv = AT[:, :, :, n2].rearrange("p k c -> p c k")
                        if n2 % 2 == 0:
                            cx.op("act", lambda: nc.scalar.copy(out=dstv, in_=src), r=[pn], w=["ATF"])
                        else:
                            cx.op("dve", lambda: nc.vector.tensor_copy(out=dstv, in_=src), r=[pn], w=["ATF"])
                    for hf in range(2):
                        for kk1 in range(64):
                            k1 = hf * 64 + kk1
                            p = pf[pi % 4]; pn = "pfF%d" % (pi % 4); pi += 1
                            cx.op("pe", lambda: nc.tensor.matmul(p[0:64, 0:256], lhsT=AT[:, k1, 0, :], rhs=ccs[:], start=True, stop=False), r=["ATF", "ccs"], w=[pn], signal=False)
                            cx.op("pe", lambda: nc.tensor.matmul(p[0:64, 0:256], lhsT=AT[:, k1, 1, :], rhs=msc[:], start=False, stop=True), r=["ATF", "msc"], w=[pn])
                            if k1 % 2 == 0:
                                cx.op("act", lambda: nc.scalar.copy(out=W[:, kk1, :], in_=p[0:64, 0:256]), r=[pn], w=["WF"])
                            else:
                                cx.op("dve", lambda: nc.vector.tensor_copy(out=W[:, kk1, :], in_=p[0:64, 0:256]), r=[pn], w=["WF"])
                        for kb in range(8):
                            p = pf[pi % 4]; pn = "pfF%d" % (pi % 4); pi += 1
                            for kk in range(8):
                                kk1 = kb * 8 + kk
                                k1 = hf * 64 + kk1
                                cx.op("pe", lambda: nc.tensor.matmul(p[:, kk * 64:(kk + 1) * 64], lhsT=W[:, kk1, 0:128], rhs=gv[:, k1, 0, :], start=True, stop=False), r=["WF", "gtab"], w=[pn], signal=False)
                                cx.op("pe", lambda: nc.tensor.matmul(p[:, kk * 64:(kk + 1) * 64], lhsT=W[:, kk1, 128:256], rhs=gv[:, k1, 1, :], start=False, stop=True), r=["WF", "gtab"], w=[pn], signal=(kk == 7))
                            k1b = hf * 64 + kb * 8
                            dstv = YT[:].rearrange("p (k2 k1) -> p k1 k2", k1=128)[:, k1b:k1b + 8, :]
                            src = p[:, :].rearrange("p (a b) -> p a b", a=8)
                            if kb % 2 == 0:
                                cx.op("act", lambda: nc.scalar.copy(out=dstv, in_=src), r=[pn], w=["YTF"])
                            else:
                                cx.op("dve", lambda: nc.vector.tensor_copy(out=dstv, in_=src), r=[pn], w=["YTF"])
                    cx.dma("sp", YY[2, g * 128:(g + 1) * 128, CTX:], YT[:], r=["YTF"], w=["YY2_%d" % g])
                if l == 0:
                    cs256 = sb(st, "cs256", [128, 2, 512], BF16)
                    cx.dma("pool", cs256[:].rearrange("p a b -> p (a b)"), cs256_in, w=["cs256"])
                    Xc = sb(st, "XcF", [128, 2, 512], BF16)
                    cx.dma("sp", Xc[:], FX[0:CTX, :].rearrange("(a p) c -> p a c", p=128), r=G("FX", 0, CTX), w=["XcF"])
                    AB = sb(st, "ABF", [128, 512], BF16); yc = sb(st, "ycF", [128, 256], BF16)
                    sc_ = 1.0 / math.sqrt(256.0 * 128.0)
                    for g in range(4):
                        p = pf[pi % 4]; pn = "pfF%d" % (pi % 4); pi += 1
                        for a in range(2):
                            cx.op("pe", lambda: nc.tensor.matmul(p[:, :], lhsT=Xc[:, a, g * 128:(g + 1) * 128], rhs=cs256[:, a, :], start=(a == 0), stop=(a == 1)), r=["XcF", "cs256"], w=[pn], signal=(a == 1))
                        cx.op("dve", lambda: nc.vector.tensor_copy(out=AB[:], in_=p[:, :]), r=[pn], w=["ABF"])
                        p2 = pf[pi % 4]; pn2 = "pfF%d" % (pi % 4); pi += 1
                        cx.op("pe", lambda: nc.tensor.matmul(p2[:, 0:256], lhsT=ccs[:, 0:128], rhs=AB[:, 0:256], start=True, stop=False), r=["ccs", "ABF"], w=[pn2], signal=False)
                        cx.op("pe", lambda: nc.tensor.matmul(p2[:, 0:256], lhsT=msc[:, 0:128], rhs=AB[:, 256:512], start=False, stop=True), r=["msc", "ABF"], w=[pn2])
                        cx.op("act", lambda: nc.scalar.activation(out=yc[:], in_=p2[:, 0:256], func=AF.Copy, scale=sc_), r=[pn2], w=["ycF"])
                        cx.dma("sp", YY[2, g * 128:(g + 1) * 128, 0:CTX], yc[:], r=["ycF"], w=["YY2c_%d" % g])

        def phase_F(l):
            with ExitStack() as st:
                wbr = sb(st, "wbr", [128, 16, 1024], BF16); wmg = sb(st, "wmg", [128, 8, 4096], BF16); wo = sb(st, "woF", [128, 8, 1024], BF16)
                bmg = sb(st, "bmg", [128, 32], F32)
                cx.dma("sp", bmg[:], b_mgateT[l], w=["bmg"])
                wbv = w_branch[l].rearrange("b (c p) n -> p (b c) n", p=128)
                for q in range(16):
                    cx.dma("pool", wbr[:, q, :], wbv[:, q, :], w=["wbr"] if q == 0 else (), wadd=() if q == 0 else ["wbr"])
                wmv = w_mgate[l].rearrange("(k p) n -> p k n", p=128)
                for k in range(8):
                    cx.dma("pool", wmg[:, k, :], wmv[:, k, :], w=["wmg"] if k == 0 else (), wadd=() if k == 0 else ["wmg"])
                wov = w_out[l].rearrange("(k p) n -> p k n", p=128)
                for k in range(8):
                    cx.dma("pool", wo[:, k, :], wov[:, k, :], w=["woF"] if k == 0 else (), wadd=() if k == 0 else ["woF"])
                NB = 256
                hTs = [sb(st, "hTF%d" % i, [128, 8, NB], BF16) for i in range(2)]
                ys = [sb(st, "yF%d" % i, [128, 16, NB], BF16) for i in range(2)]
                xts = [sb(st, "xtF%d" % i, [128, 8, NB], F32) for i in range(2)]
                sacc = sb(st, "saccF", [128, 8, NB], F32); sT = sb(st, "sTF", [128, 8, NB], BF16)
                sig = [sb(st, "sigF%d" % i, [128, NB], F32) for i in range(2)]
                tm = [sb(st, "tmF%d" % i, [128, NB], F32) for i in range(2)]
                pG = [ps(st, "pGF%d" % i) for i in range(2)]
                pP = [ps(st, "pPF%d" % i) for i in range(2)]
                pW = [ps(st, "pWF%d" % i) for i in range(2)]
                lo = 0 if l == 0 else CTX
                YYv = YY.rearrange("b (c p) t -> p (b c) t", p=128)
                ci = 0
                for bi, t0 in enumerate(range(lo, T, NB)):
                    n = NB
                    mi = 1 if t0 < CTX else 0
                    hT = hTs[bi % 2]; y = ys[bi % 2]; xt = xts[bi % 2]; tg = "F%d" % (bi % 2)
                    yr = []
                    for q4 in range(4):
                        yr += G("YY0h%d" % q4, t0, n) + G("YY3g%d" % q4, t0, n) + ["YY1_%d" % q4, "YY2_%d" % q4, "YY2c_%d" % q4]
                    cx.dma("sp", hT[:], HTv[:, :, t0:t0 + n], r=G("HT", t0, n), w=["hT" + tg])
                    cx.dma("sp", y[:], YYv[:, :, t0:t0 + n], r=yr, w=["y" + tg])
                    cx.dma("sp", xt[:], XTv[:, :, t0:t0 + n], r=G("XT", t0, n), w=["xt" + tg])
                    for dc in range(8):
                        for br in range(4):
                            g_ = pG[ci % 2]; gn = "pGF%d" % (ci % 2); p_ = pP[ci % 2]; pn = "pPF%d" % (ci % 2)
                            sg_ = sig[ci % 2]; sgn = "sigF%d" % (ci % 2); tm_ = tm[ci % 2]; tmn = "tmF%d" % (ci % 2); ci += 1
                            col = br * 1024 + dc * 128
                            for k in range(8):
                                cx.op("pe", lambda: nc.tensor.matmul(g_[:, :n], lhsT=wmg[:, k, col:col + 128], rhs=hT[:, k, :], start=(k == 0), stop=(k == 7)),
                                      r=["wmg", "hT" + tg], w=[gn], signal=(k == 7))
                            for c in range(4):
                                cx.op("pe", lambda: nc.tensor.matmul(p_[:, :n], lhsT=wbr[:, br * 4 + c, dc * 128:(dc + 1) * 128], rhs=y[:, br * 4 + c, :], start=(c == 0), stop=(c == 3)),
                                      r=["wbr", "y" + tg], w=[pn], signal=(c == 3))
                            cx.op("act", lambda: nc.scalar.activation(out=sg_[:, :n], in_=g_[:, :n], func=AF.Sigmoid, bias=bmg[:, br * 8 + dc:br * 8 + dc + 1], scale=1.0),
                                  r=[gn, "bmg"], w=[sgn])
                            if br == 0:
                                cx.op("dve", lambda: nc.vector.tensor_tensor(out=sacc[:, dc, :], in0=p_[:, :n], in1=sg_[:, :n], op=ALU.mult), r=[pn, sgn], w=["saccF"])
                            else:
                                cx.op("dve", lambda: nc.vector.tensor_tensor(out=tm_[:, :n], in0=p_[:, :n], in1=sg_[:, :n], op=ALU.mult), r=[pn, sgn], w=[tmn])
                                if br < 3:
                                    cx.op("pool", lambda: nc.gpsimd.tensor_tensor(out=sacc[:, dc, :], in0=sacc[:, dc, :], in1=tm_[:, :n], op=ALU.add), r=["saccF", tmn], w=["saccF"])
                                else:
                                    cx.op("pool", lambda: nc.gpsimd.tensor_tensor(out=sT[:, dc, :], in0=sacc[:, dc, :], in1=tm_[:, :n], op=ALU.add), r=["saccF", tmn], w=["sTF"])
                    for dc in range(8):
                        w_ = pW[dc % 2]; wn = "pWF%d" % (dc % 2)
                        for k in range(8):
                            cx.op("pe", lambda: nc.tensor.matmul(w_[:, :n], lhsT=wo[:, k, dc * 128:(dc + 1) * 128], rhs=sT[:, k, :], start=(k == 0), stop=(k == 7)),
                                  r=["woF", "sTF"], w=[wn], signal=(k == 7))
                        cx.op("dve", lambda: nc.vector.scalar_tensor_tensor(out=xt[:, dc, :], in0=w_[:, :n], scalar=gt1[:, mi, dc:dc + 1], in1=xt[:, dc, :], op0=ALU.mult, op1=ALU.add),
                              r=[wn, "modv", "xt" + tg], w=["xt" + tg])
                    cx.dma("sp", XTv[:, :, t0:t0 + n], xt[:], r=["xt" + tg], w=G("XT", t0, n))

        def phase_G(l):
            with ExitStack() as st:
                wr = sb(st, "wrG", [128, 8, NE], F32); br_ = sb(st, "brG", [128, NE], F32)
                cx.dma("sp", wr[:], w_router[l].rearrange("(k p) e -> p k e", p=128), w=["wrG"])
                cx.dma("sp", br_[:], b_router_rep[l], w=["brG"])
                xts = [sb(st, "xtG%d" % i, [128, 8, 512], F32) for i in range(2)]
                hTs = [sb(st, "hTG%d" % i, [128, 8, 512], BF16) for i in range(2)]
                h32 = sb(st, "h32G", [128, 8, 512], F32)
                sq = sb(st, "sqG", [128, 8, 512], BF16); R = sb(st, "RG", [128, 512], F32); tmp = sb(st, "tmpG", [128, 8, 512], F32)
                lg = sb(st, "lgG", [128, NE], F32); m8 = sb(st, "m8G", [128, 8], F32); msk = sb(st, "mskG", [128, NE], F32)
                nm = sb(st, "nmG", [128, 1], F32); ex = sb(st, "exG", [128, NE], F32); sm = sb(st, "smG", [128, 1], F32)
                cb = sb(st, "cbG", [128, NE], F32); cT = [sb(st, "cTG%d" % i, [NE, 512], F32) for i in range(2)]
                pss = ps(st, "pssG"); pl_ = [ps(st, "plG%d" % i) for i in range(2)]; pt_ = ps(st, "ptG")
                for bi, (t0, n) in enumerate(blocks512(l)):
                    mi = 1 if t0 < CTX else 0
                    xt = xts[bi % 2]; hT = hTs[bi % 2]; tg = "G%d" % (bi % 2)
                    cx.dma("sp", xt[:, :, :n], XTv[:, :, t0:t0 + n], r=G("XT", t0, n), w=["xt" + tg])
                    norm_block((sq, pss, R, tmp), xt, n, gs2, sh2, mi, hT, h32=h32, tag=tg)
                    cx.dma("sp", H2Tv[:, :, t0:t0 + n], hT[:, :, :n], r=["hT" + tg], w=G("H2T", t0, n))
                    ct = cT[bi % 2]; ctn = "cTG%d" % (bi % 2)
                    for j in range(n // 128):
                        p = pl_[j % 2]; pn = "plG%d" % (j % 2)
                        for k in range(8):
                            cx.op("pe", lambda: nc.tensor.matmul(p[:, 0:NE], lhsT=h32[:, k, j * 128:(j + 1) * 128], rhs=wr[:, k, :], start=(k == 0), stop=(k == 7)),
                                  r=["h32", "wrG"], w=[pn], signal=(k == 7))
                        cx.op("dve", lambda: nc.vector.tensor_tensor(out=lg[:], in0=p[:, 0:NE], in1=br_[:], op=ALU.add), r=[pn, "brG"], w=["lgG"])
                        cx.op("dve", lambda: nc.vector.max(out=m8[:], in_=lg[:]), r=["lgG"], w=["m8G"])
                        cx.op("dve", lambda: nc.vector.tensor_scalar(out=msk[:], in0=lg[:], scalar1=m8[:, 3:4], scalar2=None, op0=ALU.is_ge), r=["lgG", "m8G"], w=["mskG"])
                        cx.op("dve", lambda: nc.vector.tensor_scalar(out=nm[:], in0=m8[:, 0:1], scalar1=-1.0, scalar2=None, op0=ALU.mult), r=["m8G"], w=["nmG"])
                        cx.op("act", lambda: nc.scalar.activation(out=ex[:], in_=lg[:], func=AF.Exp, bias=nm[:, 0:1], scale=1.0), r=["lgG", "nmG"], w=["exG"])
                        cx.op("dve", lambda: nc.vector.tensor_tensor(out=ex[:], in0=ex[:], in1=msk[:], op=ALU.mult), r=["exG", "mskG"], w=["exG"])
                        cx.op("dve", lambda: nc.vector.reduce_sum(out=sm[:], in_=ex[:], axis=AX.X), r=["exG"], w=["smG"])
                        cx.op("dve", lambda: nc.vector.reciprocal(out=sm[:], in_=sm[:]), r=["smG"], w=["smG"])
                        cx.op("dve", lambda: nc.vector.tensor_scalar(out=cb[:], in0=ex[:], scalar1=sm[:, 0:1], scalar2=None, op0=ALU.mult), r=["exG", "smG"], w=["cbG"])
                        cx.op("pe", lambda: nc.tensor.transpose(pt_[0:NE, 0:128], cb[:], ident[:]), r=["cbG", "ident"], w=["ptG"])
                        cx.op("act", lambda: nc.scalar.copy(out=ct[:, j * 128:(j + 1) * 128], in_=pt_[0:NE, 0:128]), r=["ptG"], w=[ctn])
                    cx.dma("sp", COMBT[:, t0:t0 + n], ct[:, :n], r=[ctn], w=G("COMBT", t0, n))

        def phase_H(l):
            with ExitStack() as st:
                bg = sb(st, "bgH", [128, NE * 8], F32); bu = sb(st, "buH", [128, NE * 8], F32); bd = sb(st, "bdH", [NE, D], F32)
                cx.dma("sp", bg[:], b_egT[l], w=["bgH"]); cx.dma("sp", bu[:], b_euT[l], w=["buH"]); cx.dma("sp", bd[:], b_ed[l], w=["bdH"])
                cx.op("dve", lambda: nc.vector.tensor_scalar(out=bu[:], in0=bu[:], scalar1=1.0, scalar2=None, op0=ALU.add), r=["buH"], w=["buH"])
                SBK = 1024
                wg = [sb(st, "wgH%d" % i, [128, 8, D], BF16) for i in range(2)]
                wu = [sb(st, "wuH%d" % i, [128, 8, D], BF16) for i in range(2)]
                wd = [sb(st, "wdH%d" % i, [128, 8, D], BF16) for i in range(2)]
                h2s = [sb(st, "h2H%d" % i, [128, 8, SBK], BF16) for i in range(2)]; acc = sb(st, "accH", [128, 8, SBK], F32)
                cT = sb(st, "cTH", [NE, SBK], F32)
                aT = [sb(st, "aTH%d" % i, [128, 8, 512], BF16) for i in range(2)]
                cB = [sb(st, "cBH%d" % i, [128, 512], F32) for i in range(2)]
                gt = [sb(st, "gtH%d" % i, [128, 512], F32) for i in range(2)]
                yt_ = [sb(st, "ytH%d" % i, [128, 512], F32) for i in range(2)]
                ut = [sb(st, "utH%d" % i, [128, 512], F32) for i in range(2)]
                pG = [ps(st, "pGH%d" % i) for i in range(3)]
                pU = [ps(st, "pUH%d" % i) for i in range(3)]
                pY = [ps(st, "pYH%d" % i) for i in range(2)]
                lo = 0 if l == 0 else CTX
                sblocks = []
                t = lo
                while t < T:
                    n = min(SBK, T - t); sblocks.append((t, n)); t += n
                ld = 0
                wgv = lambda e: w_eg[l, e].rearrange("(k p) n -> p k n", p=128)
                wuv = lambda e: w_eu[l, e].rearrange("(k p) n -> p k n", p=128)
                wdv = lambda e: w_ed[l, e].rearrange("(k p) n -> p k n", p=128)

                pend = []

                def sched_loads(e, slot):
                    for (wt, wv_, nm, kind) in ((wg, wgv, "wgH", "gu"), (wu, wuv, "wuH", "gu"), (wd, wdv, "wdH", "d")):
                        for k in range(0, 8, 4):
                            pend.append((kind, (lambda wt=wt, wv_=wv_, nm=nm, k=k: cx.dma(
                                "pool", wt[slot][:, k:k + 4, :], wv_(e)[:, k:k + 4, :],
                                w=["%s%d" % (nm, slot)] if k == 0 else (), wadd=() if k == 0 else ["%s%d" % (nm, slot)]))))

                def pump(allow_d):
                    if pend and (pend[0][0] == "gu" or allow_d):
                        pend.pop(0)[1]()

                def load_w(e, slot):
                    for k in range(0, 8, 4):
                        cx.dma("pool", wg[slot][:, k:k + 4, :], wgv(e)[:, k:k + 4, :], w=["wgH%d" % slot] if k == 0 else (), wadd=() if k == 0 else ["wgH%d" % slot])
                    for k in range(0, 8, 4):
                        cx.dma("pool", wu[slot][:, k:k + 4, :], wuv(e)[:, k:k + 4, :], w=["wuH%d" % slot] if k == 0 else (), wadd=() if k == 0 else ["wuH%d" % slot])
                    for k in range(0, 8, 4):
                        cx.dma("pool", wd[slot][:, k:k + 4, :], wdv(e)[:, k:k + 4, :], w=["wdH%d" % slot] if k == 0 else (), wadd=() if k == 0 else ["wdH%d" % slot])

                gi = 0
                fi = 0
                cur_slot = 0
                prev = None
                first = True
                xbufs = [(gt[0], "gtH0"), (gt[1], "gtH1"), (ut[0], "utH0"), (ut[1], "utH1")]
                xi_ = 0
                nsb = len(sblocks)
                for sbi, (s0, sn) in enumerate(sblocks):
                    h2 = h2s[sbi % 2]; h2n = "h2H%d" % (sbi % 2)
                    if sbi == 0:
                        cx.dma("sp", h2[:, :, :sn], H2Tv[:, :, s0:s0 + sn], r=G("H2T", s0, sn), w=[h2n])
                    cx.dma("sp", cT[:, :sn], COMBT[:, s0:s0 + sn], r=G("COMBT", s0, sn), w=["cTH"])
                    subs = [(q, min(512, sn - q)) for q in range(0, sn, 512)]
                    for (q, n) in subs:
                        for dc in range(8):
                            pB = pY[dc % 2]; pBn = "pYH%d" % (dc % 2)
                            cx.op("pe", lambda: nc.tensor.matmul(pB[:, :n], lhsT=bd[:, dc * 128:(dc + 1) * 128], rhs=cT[:, q:q + n], start=True, stop=True), r=["bdH", "cTH"], w=[pBn])
                            cx.op("act", lambda: nc.scalar.copy(out=acc[:, dc, q:q + n], in_=pB[:, :n]), r=[pBn], w=["accH%d" % dc])
                    work = [(e, q, n) for e in range(NE) for (q, n) in subs]
                    if first:
                        load_w(0, cur_slot)

                    def emit_GU(e, q, n, slot, par, allow_d=False):
                        nonlocal fi
                        a = aT[par]; an = "aTH%d" % par
                        cb_ = cB[par]; cbn = "cBH%d" % par
                        cx.dma("sp", cb_[:, :n], COMBT[e:e + 1, s0 + q:s0 + q + n].partition_broadcast(128), r=G("COMBT", s0 + q, n), w=[cbn])
                        for fc in range(8):
                            g_ = pG[fi % 3]; gn = "pGH%d" % (fi % 3); u_ = pU[fi % 3]; un = "pUH%d" % (fi % 3)
                            g1_ = gt[fi % 2]; g1n = "gtH%d" % (fi % 2); s1_ = g1_; s1n = g1n; u1_ = ut[fi % 2]; u1n = "utH%d" % (fi % 2)
                            fi += 1
                            for k in range(8):
                                cx.op("pe", lambda: nc.tensor.matmul(g_[:, :n], lhsT=wg[slot][:, k, fc * 128:(fc + 1) * 128], rhs=h2[:, k, q:q + n], start=(k == 0), stop=(k == 7)),
                                      r=["wgH%d" % slot, h2n], w=[gn], signal=(k == 7))
                            for k in range(8):
                                cx.op("pe", lambda: nc.tensor.matmul(u_[:, :n], lhsT=wu[slot][:, k, fc * 128:(fc + 1) * 128], rhs=h2[:, k, q:q + n], start=(k == 0), stop=(k == 7)),
                                      r=["wuH%d" % slot, h2n], w=[un], signal=(k == 7))
                            bi_ = e * 8 + fc
                            cx.op("dve", lambda: nc.vector.tensor_scalar(out=g1_[:, :n], in0=g_[:, :n], scalar1=bg[:, bi_:bi_ + 1], scalar2=LIMIT, op0=ALU.add, op1=ALU.min), r=[gn, "bgH"], w=[g1n])
                            cx.op("act", lambda: nc.scalar.activation(out=s1_[:, :n], in_=g1_[:, :n], func=AF.Gelu_apprx_sigmoid), r=[g1n], w=[g1n])
                            cx.op("dve", lambda: nc.vector.tensor_scalar(out=u1_[:, :n], in0=u_[:, :n], scalar1=bu[:, bi_:bi_ + 1], scalar2=LIMIT + 1.0, op0=ALU.add, op1=ALU.min), r=[un, "buH"], w=[u1n])
                            cx.op("dve", lambda: nc.vector.scalar_tensor_tensor(out=u1_[:, :n], in0=u1_[:, :n], scalar=1.0 - LIMIT, in1=s1_[:, :n], op0=ALU.max, op1=ALU.mult), r=[u1n, s1n], w=[u1n])
                            cx.op("pool", lambda: nc.gpsimd.tensor_tensor(out=a[:, fc, :n], in0=u1_[:, :n], in1=cb_[:, :n], op=ALU.mult), r=[u1n, cbn], w=[an])
                            if fc % 2 == 1:
                                pump(allow_d)

                    def emit_Y(e, q, n, slot, par):
                        a = aT[par]; an = "aTH%d" % par
                        for dc in range(8):
                            y_ = pY[dc % 2]; yn = "pYH%d" % (dc % 2)
                            for fc in range(8):
                                cx.op("pe", lambda: nc.tensor.matmul(y_[:, :n], lhsT=wd[slot][:, fc, dc * 128:(dc + 1) * 128], rhs=a[:, fc, :n], start=(fc == 0), stop=(fc == 7)),
                                      r=["wdH%d" % slot, an], w=[yn], signal=(fc == 7))
                            if dc % 2 == 0:
                                cx.op("dve", lambda: nc.vector.tensor_tensor(out=acc[:, dc, q:q + n], in0=y_[:, :n], in1=acc[:, dc, q:q + n], op=ALU.add), r=[yn, "accH%d" % dc], w=["accH%d" % dc])
                            else:
                                yt = yt_[(dc // 2) % 2]; ytn = "ytH%d" % ((dc // 2) % 2)
                                cx.op("act", lambda: nc.scalar.copy(out=yt[:, :n], in_=y_[:, :n]), r=[yn], w=[ytn])
                                cx.op("pool", lambda: nc.gpsimd.tensor_tensor(out=acc[:, dc, q:q + n], in0=yt[:, :n], in1=acc[:, dc, q:q + n], op=ALU.add), r=[ytn, "accH%d" % dc], w=["accH%d" % dc])

                    for wi, (e, q, n) in enumerate(work):
                        if q == 0 and not first:
                            cur_slot = 1 - cur_slot
                        par = gi % 2; gi += 1
                        if q == 0:
                            while pend:
                                pend.pop(0)[1]()
                            if e + 1 < NE:
                                sched_loads(e + 1, 1 - cur_slot)
                            elif sbi + 1 < nsb:
                                sched_loads(0, 1 - cur_slot)
                            if e == 12 and sbi + 1 < nsb:
                                (s0n, snn) = sblocks[sbi + 1]
                                cx.dma("sp", h2s[(sbi + 1) % 2][:, :, :snn], H2Tv[:, :, s0n:s0n + snn], r=G("H2T", s0n, snn), w=["h2H%d" % ((sbi + 1) % 2)])
                        first = False
                        emit_GU(e, q, n, cur_slot, par, allow_d=(q != 0))
                        if prev is not None:
                            emit_Y(*prev)
                        prev = (e, q, n, cur_slot, par)
                        if len(subs) == 1:
                            while pend:
                                pend.pop(0)[1]()
                    emit_Y(*prev)
                    prev = None
                    for (q, n) in [(qq, 256) for qq in range(0, sn, 256)]:
                        t0 = s0 + q
                        mi = 1 if t0 < CTX else 0
                        for dc in range(8):
                            xr, xrn = xbufs[xi_ % 4]; xi_ += 1
                            cx.dma("sp", xr[:, :n], XT[dc * 128:(dc + 1) * 128, t0:t0 + n], r=G("XT", t0, n), w=[xrn])
                            cx.op("dve", lambda: nc.vector.scalar_tensor_tensor(out=xr[:, :n], in0=acc[:, dc, q:q + n], scalar=gt2[:, mi, dc:dc + 1], in1=xr[:, :n], op0=ALU.mult, op1=ALU.add),
                                  r=["accH%d" % dc, "modv", xrn], w=[xrn])
                            cx.dma("sp", XT[dc * 128:(dc + 1) * 128, t0:t0 + n], xr[:, :n], r=[xrn], w=G("XT", t0, n) if dc == 0 else (), wadd=() if dc == 0 else G("XT", t0, n))
                while pend:
                    pend.pop(0)[1]()

        def phase_I():
            with ExitStack() as st:
                gf = sb(st, "gfI", [128, 8], F32)
                cx.dma("sp", gf[:], gfT, w=["gfI"])
                xts = [sb(st, "xtI%d" % i, [128, 8, 512], F32) for i in range(2)]
                sq = sb(st, "sqI", [128, 8, 512], BF16); R = sb(st, "RI", [128, 512], F32); yt = sb(st, "ytI", [128, 8, 512], F32)
                oo = [sb(st, "ooI%d" % i, [128, D], F32) for i in range(2)]
                pss = ps(st, "pssI"); pt = [ps(st, "ptI%d" % i) for i in range(4)]
                oi = 0
                for bi, (t0, n) in enumerate(blocks512(1)):
                    xt = xts[bi % 2]; tg = "I%d" % (bi % 2)
                    cx.dma("sp", xt[:, :, :n], XTv[:, :, t0:t0 + n], r=G("XT", t0, n), w=["xt" + tg])
                    for k in range(8):
                        cx.op("dve", lambda: nc.vector.tensor_tensor(out=sq[:, k, :n], in0=xt[:, k, :n], in1=xt[:, k, :n], op=ALU.mult), r=["xt" + tg], w=["sq"])
                    for k in range(8):
                        cx.op("pe", lambda: nc.tensor.matmul(pss[:, :n], lhsT=ones_bf[:], rhs=sq[:, k, :n], start=(k == 0), stop=(k == 7)), r=["sq", "ones"], w=["pss"], signal=(k == 7))
                    cx.op("act", lambda: nc.scalar.activation(out=R[:, :n], in_=pss[:, :n], func=AF.Sqrt, scale=1.0 / D, bias=epsc[:, 0:1]), r=["pss", "epsc"], w=["R"])
                    cx.op("dve", lambda: nc.vector.reciprocal(out=R[:, :n], in_=R[:, :n]), r=["R"], w=["R"])
                    for k in range(8):
                        cx.op("dve", lambda: nc.vector.scalar_tensor_tensor(out=yt[:, k, :n], in0=xt[:, k, :n], scalar=gf[:, k:k + 1], in1=R[:, :n], op0=ALU.mult, op1=ALU.mult),
                              r=["xt" + tg, "R", "gfI"], w=["ytI"])
                    for j in range(n // 128):
                        o = oo[oi % 2]; on = "ooI%d" % (oi % 2); oi += 1
                        for hf in range(2):
                            p = pt[(j % 2) * 2 + hf]; pn = "ptI%d" % ((j % 2) * 2 + hf)
                            for q in range(4):
                                k = hf * 4 + q
                                cx.op("pe", lambda: nc.tensor.transpose(p[:, q * 128:(q + 1) * 128], yt[:, k, j * 128:(j + 1) * 128], ident[:]), r=["ytI", "ident"], w=[pn], signal=(q == 3))
                            if hf == 0:
                                cx.op("act", lambda: nc.scalar.copy(out=o[:, 0:512], in_=p[:, :]), r=[pn], w=[on + "a"])
                            else:
                                cx.op("dve", lambda: nc.vector.tensor_copy(out=o[:, 512:1024], in_=p[:, :]), r=[pn], w=[on + "b"])
                        r0_ = t0 - CTX + j * 128
                        cx.dma("sp", out_ap[r0_:r0_ + 128, :], o[:], r=[on + "a", on + "b"], wadd=["OUT"])

        order = []
        order.append(("B", phase_B))
        for l in range(layers):
            order += [("A%d" % l, lambda l=l: phase_A(l)), ("C%d" % l, lambda l=l: phase_C(l)), ("D%d" % l, lambda l=l: phase_D(l)),
                      ("Ec%d" % l, lambda l=l: phase_E_conv(l)), ("Ep%d" % l, lambda l=l: phase_E_pool(l)), ("Ef%d" % l, lambda l=l: phase_E_fourier(l)),
                      ("F%d" % l, lambda l=l: phase_F(l)), ("G%d" % l, lambda l=l: phase_G(l)), ("H%d" % l, lambda l=l: phase_H(l))]
        order.append(("I", phase_I))
        for name, fn in order:
            cx.barrier()
            fn()
            if stop_after is not None and name == stop_after:
                break
        cx.finish()
    return nc


def _colT(v, nchunk):
    return np.ascontiguousarray(np.swapaxes(v.reshape(v.shape[:-1] + (nchunk, 128)), -1, -2))


def make_in_maps(inputs, n_cores=8):
    f = lambda a: np.ascontiguousarray(np.asarray(a, dtype=np.float32))
    x = f(inputs["x"]); c = f(inputs["c"]); ctx = f(inputs["ctx"]); c_ctx = f(inputs["c_ctx"])
    w_in = f(inputs["w_in"])
    perm = _rot_perm()
    cols = []
    for which in range(2):
        for h in range(4):
            for i in range(2):
                base = which * 512 + h * 128 + i * 64
                cols.append(base + perm)
    cols = np.concatenate(cols)
    w_in_rot = np.ascontiguousarray(w_in[:, :, cols])
    cosT, sinT = _rope_tables()
    cs128, ccs, msc, g, cs256 = _dft_tables()
    shared = {
        "w_ada": f(inputs["w_ada"]), "b_adaT": _colT(f(inputs["b_ada"]), 48),
        "g1T": _colT(f(inputs["g_norm1"]), 8), "g2T": _colT(f(inputs["g_norm2"]), 8), "gfT": _colT(f(inputs["g_final"]), 8),
        "w_in": w_in, "w_in_rot": w_in_rot,
        "lam_rep": np.ascontiguousarray(np.broadcast_to(f(inputs["da_lambda"]).reshape(L, 1, 256), (L, 128, 256))),
        "gsubT": np.ascontiguousarray(f(inputs["g_subln"]).reshape(L, 128, 1)),
        "w_convT": np.ascontiguousarray(f(inputs["w_conv"]).reshape(L, 3, 4, 128).transpose(0, 3, 2, 1).reshape(L, 128, 12)),
        "w_pool": f(inputs["w_pool"]), "pool_scaleT": _colT(f(inputs["pool_scale"]), 4),
        "w_branch": f(inputs["w_branch"]), "w_mgate": f(inputs["w_mgate"]), "b_mgateT": _colT(f(inputs["b_mgate"]), 32),
        "w_out": f(inputs["w_out"]), "w_router": f(inputs["w_router"]),
        "b_router_rep": np.ascontiguousarray(np.broadcast_to(f(inputs["b_router"]).reshape(L, 1, NE), (L, 128, NE))),
        "w_e_gate": f(inputs["w_e_gate"]), "w_e_up": f(inputs["w_e_up"]), "w_e_down": f(inputs["w_e_down"]),
        "b_egT": np.ascontiguousarray(_colT(f(inputs["b_e_gate"]), 8).transpose(0, 2, 1, 3).reshape(L, 128, NE * 8)),
        "b_euT": np.ascontiguousarray(_colT(f(inputs["b_e_up"]), 8).transpose(0, 2, 1, 3).reshape(L, 128, NE * 8)),
        "b_e_down": f(inputs["b_e_down"]),
        "rope_cos": cosT, "rope_sin": sinT, "ident": np.eye(128, dtype=np.float32),
        "cs128": cs128, "ccs": ccs, "msc": msc, "gtab": g, "cs256": cs256, "invcnt": _invcnt_tables(),
    }
    maps = []
    for b in range(n_cores):
        m = dict(shared)
        m["x"] = x[b]; m["ctx"] = ctx[b]
        m["cvec"] = np.ascontiguousarray(np.concatenate([_colT(c[b], 8), _colT(c_ctx, 8)], axis=1))
        maps.append(m)
    return maps


def kernel(**inputs):
    nc = build_program()
    maps = make_in_maps(inputs, 8)
    res = run_bass_kernel_spmd(nc, maps, core_ids=list(range(8)))
    return np.stack([np.asarray(r["out"], dtype=np.float32) for r in res.results], axis=0)
```

```python
import os
import math
from contextlib import ExitStack
from collections import defaultdict
import numpy as np
import concourse.bass as bass
import concourse.mybir as mybir
from concourse.bass_utils import run_bass_kernel_spmd

F32, BF16 = mybir.dt.float32, mybir.dt.bfloat16
AF = mybir.ActivationFunctionType
ALU = mybir.AluOpType
AX = mybir.AxisListType

D = 1024; S = 8192; CTX = 256; T = S + CTX; L = 2; NE = 32; EPS = 1e-6
GRID_W = 64
LIMIT = 7.0; ALPHA = 1.702
POOL_WINDOWS = (2, 4, 8, 16)


class Buf:
    __slots__ = ("w", "r")

    def __init__(self):
        self.w = {}
        self.r = {}


class Cx:
    def __init__(self, nc, stack):
        self.nc = nc
        self.engs = {"pe": nc.tensor, "dve": nc.vector, "act": nc.scalar, "pool": nc.gpsimd, "sp": nc.sync}
        self.sems = {}
        self.cnt = {}
        for k in self.engs:
            self.sems[k] = stack.enter_context(nc.semaphore("s_" + k))
            self.cnt[k] = 0
        self.dkeys = {"sp": [], "pool": []}
        for q, n in (("sp", 16), ("pool", 1)):
            for i in range(n):
                key = "d%s%d" % (q, i)
                self.sems[key] = stack.enter_context(nc.semaphore("s_" + key))
                self.cnt[key] = 0
                self.dkeys[q].append(key)
        self.rr = {"sp": 0, "pool": 0}
        self.seen = {k: {} for k in self.engs}
        self.hist = {k: [] for k in ("pe", "dve", "act", "pool")}
        self.pending = {k: [] for k in self.engs}
        self.B = defaultdict(Buf)

    def _wait(self, E, key, c):
        if key == E:
            if E in ("pe", "sp") or c < self.cnt[E]:
                return
        if self.seen[E].get(key, 0) >= c:
            return
        self.engs[E].wait_ge(self.sems[key], c)
        self.seen[E][key] = c
        if key in self.hist:
            h = self.hist[key]
            lo, hi = 0, len(h)
            while lo < hi:
                mid = (lo + hi) // 2
                if h[mid][0] < c:
                    lo = mid + 1
                else:
                    hi = mid
            if lo > 0:
                for k2, c2 in h[lo - 1][1].items():
                    if k2 != E and self.seen[E].get(k2, 0) < c2:
                        self.seen[E][k2] = c2
        if E in self.hist:
            self.hist[E].append((self.cnt[E], dict(self.seen[E])))

    def _deps(self, E, reads, writes):
        for b in reads:
            for k, c in list(b.w.items()):
                self._wait(E, k, c)
        for b in writes:
            for k, c in list(b.w.items()):
                self._wait(E, k, c)
            for k, c in list(b.r.items()):
                self._wait(E, k, c)

    def bufs(self, names):
        return [self.B[n] for n in names]

    def op(self, E, fn, r=(), w=(), signal=True):
        reads = self.bufs(r); writes = self.bufs(w)
        self._deps(E, reads, writes)
        inst = fn()
        self.pending[E].append((reads, writes))
        if signal:
            self.cnt[E] += 1
            c = self.cnt[E]
            inst.then_inc(self.sems[E], 1)
            for rs, ws in self.pending[E]:
                for b in rs:
                    b.r[E] = c
                for b in ws:
                    b.w = {E: c}
                    b.r = {}
            self.pending[E] = []
        return inst

    def dma(self, Q, out, in_, r=(), w=(), wadd=()):
        reads = self.bufs(r); writes = self.bufs(w); wadds = self.bufs(wadd)
        keys = self.dkeys[Q]
        key = keys[self.rr[Q] % len(keys)]
        self.rr[Q] += 1
        if self.cnt[key] > 0:
            self._wait(Q, key, self.cnt[key])
        self._deps(Q, reads, writes)
        for b in wadds:
            for k, c0 in list(b.r.items()):
                self._wait(Q, k, c0)
        inst = self.engs[Q].dma_start(out=out, in_=in_)
        self.cnt[key] += 16
        c = self.cnt[key]
        inst.then_inc(self.sems[key], 16)
        for b in reads:
            b.r[key] = c
        for b in writes:
            b.w = {key: c}
            b.r = {}
        for b in wadds:
            b.w[key] = c
        return inst

    def barrier(self):
        for E in self.engs:
            assert not self.pending[E]
        for E in self.engs:
            for key, c in self.cnt.items():
                if c > 0:
                    self._wait(E, key, c)

    def finish(self):
        for q in ("sp", "pool"):
            for key in self.dkeys[q]:
                if self.cnt[key] > 0:
                    self.nc.sync.wait_ge(self.sems[key], self.cnt[key])


def _rope_tables():
    half = 32
    inv = (10000.0 ** (-np.arange(0, half, 2, dtype=np.float32) / half)).astype(np.float32)
    s = np.arange(S)
    row = (s // GRID_W).astype(np.float32); col = (s % GRID_W).astype(np.float32)
    ar = row[:, None] * inv; ac = col[:, None] * inv
    ang = np.concatenate([ar, ar, ac, ac], axis=-1).astype(np.float32)
    cos = np.cos(ang).astype(np.float32); sin = np.sin(ang).astype(np.float32)
    sign = np.where((np.arange(64) // 16) % 2 == 0, -1.0, 1.0).astype(np.float32)
    sin = sin * sign[None, :]
    cosT = np.ones((128, T), np.float32); sinT = np.zeros((128, T), np.float32)
    cosT[:, CTX:] = np.concatenate([cos.T, cos.T], axis=0)
    sinT[:, CTX:] = np.concatenate([sin.T, sin.T], axis=0)
    return cosT, sinT


def _rot_perm():
    d = np.arange(64)
    return np.where((d // 16) % 2 == 0, d + 16, d - 16)


def _dft_tables():
    n1 = np.arange(128)[:, None].astype(np.float64); k1 = np.arange(128)[None, :].astype(np.float64)
    a = 2 * np.pi * n1 * k1 / 128.0
    cs128 = np.concatenate([np.cos(a), np.sin(a)], axis=1).astype(np.float32)
    ccs = cs128.copy()
    msc = np.concatenate([-np.sin(a), np.cos(a)], axis=1).astype(np.float32)
    n2 = np.arange(64)[:, None, None].astype(np.float64)
    kk1 = np.arange(128)[None, :, None].astype(np.float64)
    k2 = np.arange(64)[None, None, :].astype(np.float64)
    ph = 2 * np.pi * (n2 * kk1 / 8192.0 + n2 * k2 / 64.0)
    g = np.stack([np.cos(ph), -np.sin(ph)], axis=2) / 1024.0
    g = g.reshape(64, 128 * 2 * 64).astype(np.float32)
    t = np.arange(256)[:, None].astype(np.float64); kp = np.arange(256)[None, :].astype(np.float64)
    b = 2 * np.pi * t * kp / 256.0
    cs256 = np.concatenate([np.cos(b), np.sin(b)], axis=1).astype(np.float32)
    cs256 = cs256.reshape(2, 128, 512).transpose(1, 0, 2).reshape(128, 1024).copy()
    return cs128, ccs, msc, g, cs256


def _invcnt_tables():
    out = np.zeros((4, T), np.float32)
    for gi, wd in enumerate(POOL_WINDOWS):
        for (a, n) in ((0, CTX), (CTX, S)):
            t = np.arange(n)
            lo = np.clip(t - wd // 2, 0, n); hi = np.clip(t + wd // 2, 0, n)
            out[gi, a:a + n] = 1.0 / (hi - lo).astype(np.float32)
    return out


def build_program(debug=False, stop_after=None, layers=L, moe=True):
    nc = bass.Bass("TRN2", target_bir_lowering=False)
    dbg_kind = "ExternalOutput" if debug else "Internal"

    def din(name, shape, dt=F32):
        return nc.dram_tensor(name, list(shape), dt, kind="ExternalInput").ap()

    def dscr(name, shape, dt):
        return nc.dram_tensor(name, list(shape), dt, kind=dbg_kind).ap()

    x_in = din("x", [S, D]); ctx_in = din("ctx", [CTX, D]); cvec = din("cvec", [128, 16])
    w_ada = din("w_ada", [L, D, 6 * D]); b_adaT = din("b_adaT", [L, 128, 48])
    g1T = din("g1T", [L, 128, 8]); g2T = din("g2T", [L, 128, 8]); gfT = din("gfT", [128, 8])
    w_in = din("w_in", [L, D, 4096]); w_in_rot = din("w_in_rot", [L, D, 1024])
    lam_rep = din("lam_rep", [L, 128, 256]); gsubT = din("gsubT", [L, 128, 1])
    w_convT = din("w_convT", [L, 128, 12]); w_pool = din("w_pool", [L, 4, 128, 128])
    pool_scaleT = din("pool_scaleT", [L, 128, 4])
    w_branch = din("w_branch", [L, 4, 512, D]); w_mgate = din("w_mgate", [L, D, 4 * D])
    b_mgateT = din("b_mgateT", [L, 128, 32]); w_out = din("w_out", [L, D, D])
    w_router = din("w_router", [L, D, NE]); b_router_rep = din("b_router_rep", [L, 128, NE])
    ned = NE if moe else 1
    w_eg = din("w_e_gate", [L, ned, D, D]); w_eu = din("w_e_up", [L, ned, D, D]); w_ed = din("w_e_down", [L, ned, D, D])
    b_egT = din("b_egT", [L, 128, NE * 8]); b_euT = din("b_euT", [L, 128, NE * 8]); b_ed = din("b_e_down", [L, NE, D])
    rope_cos = din("rope_cos", [128, T]); rope_sin = din("rope_sin", [128, T])
    ident_in = din("ident", [128, 128])
    cs128_in = din("cs128", [128, 256]); ccs_in = din("ccs", [128, 256]); msc_in = din("msc", [128, 256])
    gtab_in = din("gtab", [64, 128 * 2 * 64]); cs256_in = din("cs256", [128, 1024])
    invcnt_in = din("invcnt", [4, T])
    out_ap = nc.dram_tensor("out", [S, D], F32, kind="ExternalOutput").ap()

    XT = dscr("XT", [D, T], F32)
    HT = dscr("HT", [D, T], BF16)
    QT = dscr("QT", [512, T], BF16); KT = dscr("KT", [512, T], BF16); VV = dscr("VV", [T, 512], BF16)
    SXT = dscr("SXT", [512, T], BF16); SBT = dscr("SBT", [512, T], BF16); SCT = dscr("SCT", [512, T], BF16)
    FX = dscr("FX", [T, 512], BF16); PXT = dscr("PXT", [512, T], BF16)
    YY = dscr("YY", [4, 512, T], BF16)
    H2T = dscr("H2T", [D, T], BF16)
    COMBT = dscr("COMBT", [NE, T], F32)

    XTv = XT.rearrange("(k p) t -> p k t", p=128)
    HTv = HT.rearrange("(k p) t -> p k t", p=128)
    H2Tv = H2T.rearrange("(k p) t -> p k t", p=128)

    stack = ExitStack()
    with stack:
        cx = Cx(nc, stack)
        B = cx.B

        uid = [0]

        def G(name, t0, n):
            return ["%s_%d" % (name, g) for g in range(t0 // 256, (t0 + n + 255) // 256)]

        def GA(name):
            return ["%s_%d" % (name, g) for g in range(T // 256)]

        def sb(st, name, shape, dt):
            uid[0] += 1
            return st.enter_context(nc.sbuf_tensor("%s_u%d" % (name, uid[0]), list(shape), dt))

        def ps(st, name, dt=F32, cols=512):
            uid[0] += 1
            return st.enter_context(nc.psum_tensor("%s_u%d" % (name, uid[0]), [128, cols], dt))

        ones_bf = sb(stack, "ones_bf", [128, 128], BF16)
        ident = sb(stack, "ident", [128, 128], F32)
        mods = sb(stack, "mods", [128, 48, 2], F32)
        gs1 = sb(stack, "gs1", [128, 2, 8], F32); gs2 = sb(stack, "gs2", [128, 2, 8], F32)
        sh1 = sb(stack, "sh1", [128, 2, 8], F32); sh2 = sb(stack, "sh2", [128, 2, 8], F32)
        gt1 = sb(stack, "gt1", [128, 2, 8], F32); gt2 = sb(stack, "gt2", [128, 2, 8], F32)
        cx.op("dve", lambda: nc.vector.memset(ones_bf[:], 1.0), w=["ones"])
        epsc = sb(stack, "epsc", [128, 1], F32)
        cx.op("dve", lambda: nc.vector.memset(epsc[:], EPS), w=["epsc"])
        cx.dma("sp", ident[:], ident_in, w=["ident"])

        def phase_B():
            with ExitStack() as st:
                xin = [sb(st, "xin%d" % i, [128, D], F32) for i in range(2)]
                xo = [sb(st, "xo%d" % i, [128, 8, 128], F32) for i in range(2)]
                pt = [ps(st, "ptB%d" % i) for i in range(2)]
                ntile = T // 128
                for j in range(ntile):
                    src = ctx_in[j * 128:(j + 1) * 128, :] if j < 2 else x_in[(j - 2) * 128:(j - 1) * 128, :]
                    xi = xin[j % 2]; xoo = xo[j % 2]
                    cx.dma("sp", xi[:], src, w=["xin%d" % (j % 2)])
                    for hf in range(2):
                        p = pt[hf]
                        for q in range(4):
                            k = hf * 4 + q
                            cx.op("pe", lambda: nc.tensor.transpose(p[:, q * 128:(q + 1) * 128], xi[:, k * 128:(k + 1) * 128], ident[:]),
                                  r=["xin%d" % (j % 2), "ident"], w=["ptB%d" % hf], signal=(q == 3))
                        eng = "dve" if hf == 0 else "act"
                        if hf == 0:
                            cx.op("dve", lambda: nc.vector.tensor_copy(out=xoo[:, 0:4, :], in_=p[:, :].rearrange("p (q t) -> p q t", q=4)),
                                  r=["ptB0"], w=["xo%d_0" % (j % 2)])
                        else:
                            cx.op("act", lambda: nc.scalar.copy(out=xoo[:, 4:8, :], in_=p[:, :].rearrange("p (q t) -> p q t", q=4)),
                                  r=["ptB1"], w=["xo%d_1" % (j % 2)])
                    cx.dma("sp", XTv[:, :, j * 128:(j + 1) * 128], xoo[:], r=["xo%d_0" % (j % 2), "xo%d_1" % (j % 2)], w=G("XT", j * 128, 128) if j % 2 == 0 else (), wadd=() if j % 2 == 0 else G("XT", j * 128, 128))

        def phase_A(l):
            with ExitStack() as st:
                cv = sb(st, "cv", [128, 16], F32); sc = sb(st, "scv", [128, 8, 2], F32)
                sg = sb(st, "sgv", [128, 16], F32)
                wa = [sb(st, "wa%d" % i, [128, 8, 1024], F32) for i in range(2)]
                bT = sb(st, "bT", [128, 48], F32); g1 = sb(st, "g1", [128, 8], F32); g2 = sb(st, "g2", [128, 8], F32)
                pm = ps(st, "pmA")
                cx.dma("sp", cv[:], cvec, w=["cv"])
                cx.dma("sp", bT[:], b_adaT[l], w=["bT"])
                cx.dma("sp", g1[:], g1T[l], w=["g1"])
                cx.dma("sp", g2[:], g2T[l], w=["g2"])
                cx.op("act", lambda: nc.scalar.activation(out=sg[:], in_=cv[:], func=AF.Sigmoid), r=["cv"], w=["sgv"])
                cx.op("dve", lambda: nc.vector.tensor_tensor(out=sc[:, :, 0], in0=cv[:, 0:8], in1=sg[:, 0:8], op=ALU.mult), r=["cv", "sgv"], w=["scv"])
                cx.op("dve", lambda: nc.vector.tensor_tensor(out=sc[:, :, 1], in0=cv[:, 8:16], in1=sg[:, 8:16], op=ALU.mult), r=["cv", "sgv"], w=["scv"])
                wav = w_ada[l].rearrange("(k p) n -> p k n", p=128)
                for wch in range(6):
                    wt = wa[wch % 2]
                    for k in range(8):
                        cx.dma("sp", wt[:, k, :], wav[:, k, wch * 1024:(wch + 1) * 1024], w=["wa%d" % (wch % 2)] if k == 0 else (), wadd=() if k == 0 else ["wa%d" % (wch % 2)])
                    for jj in range(8):
                        j = wch * 8 + jj
                        for k in range(8):
                            cx.op("pe", lambda: nc.tensor.matmul(pm[:, 2 * j:2 * j + 2], lhsT=wt[:, k, jj * 128:(jj + 1) * 128], rhs=sc[:, k, :],
                                                                 start=(k == 0), stop=(k == 7)),
                                  r=["wa%d" % (wch % 2), "scv"], w=["pmA"], signal=(k == 7))
                cx.op("dve", lambda: nc.vector.tensor_copy(out=mods[:].rearrange("p j c -> p (j c)"), in_=pm[:, 0:96]), r=["pmA"], w=["mods"])
                for c in range(2):
                    cx.op("dve", lambda: nc.vector.tensor_tensor(out=mods[:, :, c], in0=mods[:, :, c], in1=bT[:], op=ALU.add), r=["mods", "bT"], w=["mods"])
                for c in range(2):
                    cx.op("dve", lambda: nc.vector.scalar_tensor_tensor(out=gs1[:, c, :], in0=mods[:, 8:16, c], scalar=1.0, in1=g1[:], op0=ALU.add, op1=ALU.mult),
                          r=["mods", "g1"], w=["modv"])
                    cx.op("dve", lambda: nc.vector.scalar_tensor_tensor(out=gs2[:, c, :], in0=mods[:, 32:40, c], scalar=1.0, in1=g2[:], op0=ALU.add, op1=ALU.mult),
                          r=["mods", "g2"], w=["modv"])
                    cx.op("dve", lambda: nc.vector.tensor_copy(out=sh1[:, c, :], in_=mods[:, 0:8, c]), r=["mods"], w=["modv"])
                    cx.op("dve", lambda: nc.vector.tensor_copy(out=gt1[:, c, :], in_=mods[:, 16:24, c]), r=["mods"], w=["modv"])
                    cx.op("dve", lambda: nc.vector.tensor_copy(out=sh2[:, c, :], in_=mods[:, 24:32, c]), r=["mods"], w=["modv"])
                    cx.op("dve", lambda: nc.vector.tensor_copy(out=gt2[:, c, :], in_=mods[:, 40:48, c]), r=["mods"], w=["modv"])

        def norm_block(st_tiles, xt, n, gs, sh, mi, hT, h32=None, tag=""):
            sq, pss, R, tmp = st_tiles
            nstop = int(os.environ.get("NSTOP", "9"))
            if nstop < 1:
                return
            for k in range(8):
                cx.op("dve", lambda: nc.vector.tensor_tensor(out=sq[:, k, :n], in0=xt[:, k, :n], in1=xt[:, k, :n], op=ALU.mult), r=["xt" + tag], w=["sq"])
            if nstop < 2:
                return
            for k in range(8):
                cx.op("pe", lambda: nc.tensor.matmul(pss[:, :n], lhsT=ones_bf[:], rhs=sq[:, k, :n], start=(k == 0), stop=(k == 7)),
                      r=["sq", "ones"], w=["pss"], signal=(k == 7))
            if nstop < 3:
                return
            cx.op("act", lambda: nc.scalar.activation(out=R[:, :n], in_=pss[:, :n], func=AF.Sqrt, scale=1.0 / D, bias=epsc[:, 0:1]), r=["pss", "epsc"], w=["R"])
            if nstop < 4:
                return
            cx.op("dve", lambda: nc.vector.reciprocal(out=R[:, :n], in_=R[:, :n]), r=["R"], w=["R"])
            if nstop < 5:
                return
            for k in range(8):
                cx.op("dve", lambda: nc.vector.scalar_tensor_tensor(out=tmp[:, k, :n], in0=xt[:, k, :n], scalar=gs[:, mi, k:k + 1], in1=R[:, :n], op0=ALU.mult, op1=ALU.mult),
                      r=["xt" + tag, "R", "modv"], w=["ntmp"])
                if nstop < 6:
                    continue
                if h32 is not None:
                    cx.op("act", lambda: nc.scalar.activation(out=h32[:, k, :n], in_=tmp[:, k, :n], func=AF.Identity, bias=sh[:, mi, k:k + 1], scale=1.0),
                          r=["ntmp", "modv"], w=["h32"])
                    cx.op("pool", lambda: nc.gpsimd.tensor_copy(out=hT[:, k, :n], in_=h32[:, k, :n]), r=["h32"], w=["hT" + tag])
                else:
                    cx.op("act", lambda: nc.scalar.activation(out=hT[:, k, :n], in_=tmp[:, k, :n], func=AF.Identity, bias=sh[:, mi, k:k + 1], scale=1.0),
                          r=["ntmp", "modv"], w=["hT" + tag])

        def blocks512(lo):
            bl = []
            if lo == 0:
                bl.append((0, CTX))
            for i in range(S // 512):
                bl.append((CTX + i * 512, 512))
            return bl

        def phase_C(l):
            with ExitStack() as st:
                win = sb(st, "win", [128, 8, 4096], BF16); winr = sb(st, "winr", [128, 8, 1024], BF16)
                wv = w_in[l].rearrange("(k p) n -> p k n", p=128); wrv = w_in_rot[l].rearrange("(k p) n -> p k n", p=128)
                for k in range(8):
                    cx.dma("pool", win[:, k, :], wv[:, k, :], w=["win"] if k == 0 else (), wadd=() if k == 0 else ["win"])
                for k in range(8):
                    cx.dma("pool", winr[:, k, :], wrv[:, k, :], w=["winr"] if k == 0 else (), wadd=() if k == 0 else ["winr"])
                xts = [sb(st, "xtC%d" % i, [128, 8, 512], F32) for i in range(2)]
                hTs = [sb(st, "hTC%d" % i, [128, 8, 512], BF16) for i in range(2)]
                sq = sb(st, "sqC", [128, 8, 512], BF16); R = sb(st, "RC", [128, 512], F32); tmp = sb(st, "tmpC", [128, 8, 512], F32)
                cosb = [sb(st, "cosb%d" % i, [128, 512], F32) for i in range(2)]
                sinb = [sb(st, "sinb%d" % i, [128, 512], F32) for i in range(2)]
                t1 = [sb(st, "t1C%d" % i, [128, 512], F32) for i in range(2)]
                t2 = [sb(st, "t2C%d" % i, [128, 512], F32) for i in range(2)]
                stg = [sb(st, "stgC%d" % i, [128, 4, 512], BF16) for i in range(3)]
                stgt = [sb(st, "stgT%d" % i, [128, 512], BF16) for i in range(3)]
                pss = ps(st, "pssC")
                pp = [ps(st, "ppC%d" % i) for i in range(6)]
                bl = blocks512(0)
                stg_i = 0; stgt_i = 0; pp_i = 0
                cstop = int(os.environ.get("CSTOP", "9"))
                nblk = int(os.environ.get("CBLK", "99"))
                for bi, (t0, n) in enumerate(bl):
                    if bi >= nblk:
                        break
                    mi = 1 if t0 < CTX else 0
                    xt = xts[bi % 2]; hT = hTs[bi % 2]; tg = "C%d" % (bi % 2)
                    cx.dma("sp", xt[:, :, :n], XTv[:, :, t0:t0 + n], r=G("XT", t0, n), w=["xt" + tg])
                    cx.dma("sp", cosb[bi % 2][:, :n], rope_cos[:, t0:t0 + n], w=["cosb%d" % (bi % 2)])
                    cx.dma("sp", sinb[bi % 2][:, :n], rope_sin[:, t0:t0 + n], w=["sinb%d" % (bi % 2)])
                    norm_block((sq, pss, R, tmp), xt, n, gs1, sh1, mi, hT, tag=tg)
                    cx.dma("sp", HTv[:, :, t0:t0 + n], hT[:, :, :n], r=["hT" + tg], w=G("HT", t0, n))

                    def proj(wt, wname, c0, p, pname):
                        for k in range(8):
                            cx.op("pe", lambda: nc.tensor.matmul(p[:, :n], lhsT=wt[:, k, c0:c0 + 128], rhs=hT[:, k, :n], start=(k == 0), stop=(k == 7)),
                                  r=[wname, "hT" + tg], w=[pname], signal=(k == 7))
                    if cstop < 1:
                        continue
                    for which, dst, dname in ((0, QT, "QT"), (1, KT, "KT")):
                        sg_ = stg[stg_i % 3]; sname = "stgC%d" % (stg_i % 3); stg_i += 1
                        for h in range(4):
                            pa = pp[pp_i % 6]; pan = "ppC%d" % (pp_i % 6); pp_i += 1
                            pb = pp[pp_i % 6]; pbn = "ppC%d" % (pp_i % 6); pp_i += 1
                            proj(win, "win", which * 512 + h * 128, pa, pan)
                            proj(winr, "winr", which * 512 + h * 128, pb, pbn)
                            a1 = t1[h % 2]; a2 = t2[h % 2]
                            cx.op("dve", lambda: nc.vector.tensor_tensor(out=a1[:, :n], in0=pa[:, :n], in1=cosb[bi % 2][:, :n], op=ALU.mult),
                                  r=[pan, "cosb%d" % (bi % 2)], w=["t1C%d" % (h % 2)])
                            cx.op("dve", lambda: nc.vector.tensor_tensor(out=a2[:, :n], in0=pb[:, :n], in1=sinb[bi % 2][:, :n], op=ALU.mult),
                                  r=[pbn, "sinb%d" % (bi % 2)], w=["t2C%d" % (h % 2)])
                            cx.op("pool", lambda: nc.gpsimd.tensor_tensor(out=sg_[:, h, :n], in0=a1[:, :n], in1=a2[:, :n], op=ALU.add),
                                  r=["t1C%d" % (h % 2), "t2C%d" % (h % 2)], w=[sname])
                        cx.dma("sp", dst.rearrange("(h p) t -> p h t", p=128)[:, :, t0:t0 + n], sg_[:, :, :n], r=[sname], w=G(dname, t0, n))
                    if cstop < 2:
                        continue
                    for c0, dst, dname in ((1536, SXT, "SXT"), (2048, SBT, "SBT"), (2560, SCT, "SCT"), (3584, PXT, "PXT")):
                        sg_ = stg[stg_i % 3]; sname = "stgC%d" % (stg_i % 3); stg_i += 1
                        for h in range(4):
                            pa = pp[pp_i % 6]; pan = "ppC%d" % (pp_i % 6); pp_i += 1
                            proj(win, "win", c0 + h * 128, pa, pan)
                            if h % 2 == 0:
                                cx.op("act", lambda: nc.scalar.copy(out=sg_[:, h, :n], in_=pa[:, :n]), r=[pan], w=[sname])
                            else:
                                cx.op("dve", lambda: nc.vector.tensor_copy(out=sg_[:, h, :n], in_=pa[:, :n]), r=[pan], w=[sname])
                        cx.dma("sp", dst.rearrange("(h p) t -> p h t", p=128)[:, :, t0:t0 + n], sg_[:, :, :n], r=[sname], w=G(dname, t0, n))
                    if cstop < 3:
                        continue
                    for c0, dst, dname in ((1024, VV, "VV"), (3072, FX, "FX")):
                        for j in range(n // 128):
                            pa = pp[pp_i % 6]; pan = "ppC%d" % (pp_i % 6); pp_i += 1
                            so = stgt[stgt_i % 3]; son = "stgT%d" % (stgt_i % 3); stgt_i += 1
                            for k in range(8):
                                cx.op("pe", lambda: nc.tensor.matmul(pa[:, :], lhsT=hT[:, k, j * 128:(j + 1) * 128], rhs=win[:, k, c0:c0 + 512], start=(k == 0), stop=(k == 7)),
                                      r=["win", "hT" + tg], w=[pan], signal=(k == 7))
                            if j % 2 == 0:
                                cx.op("act", lambda: nc.scalar.copy(out=so[:], in_=pa[:, :]), r=[pan], w=[son])
                            else:
                                cx.op("dve", lambda: nc.vector.tensor_copy(out=so[:], in_=pa[:, :]), r=[pan], w=[son])
                            cx.dma("sp", dst[t0 + j * 128:t0 + (j + 1) * 128, :], so[:], r=[son], w=G(dname, t0, n) if j == 0 else (), wadd=() if j == 0 else G(dname, t0, n))

        def phase_D(l):
            lam_init = 0.8 - 0.6 * math.exp(-0.3 * l)
            with ExitStack() as st:
                lamt = sb(st, "lamt", [128, 256], F32); lp = sb(st, "lp", [128, 128], F32); l2 = sb(st, "l2", [128, 2], F32)
                nlam = sb(st, "nlam", [128, 1], F32); gsub = sb(st, "gsub", [128, 1], F32)
                cx.dma("sp", lamt[:], lam_rep[l], w=["lamt"])
                cx.dma("sp", gsub[:], gsubT[l], w=["gsub"])
                cx.op("dve", lambda: nc.vector.tensor_tensor(out=lp[:, 0:64], in0=lamt[:, 0:64], in1=lamt[:, 64:128], op=ALU.mult), r=["lamt"], w=["lp"])
                cx.op("dve", lambda: nc.vector.tensor_tensor(out=lp[:, 64:128], in0=lamt[:, 128:192], in1=lamt[:, 192:256], op=ALU.mult), r=["lamt"], w=["lp"])
                cx.op("dve", lambda: nc.vector.reduce_sum(out=l2[:, :], in_=lp[:].rearrange("p (a b) -> p a b", a=2), axis=AX.X), r=["lp"], w=["l2"])
                cx.op("act", lambda: nc.scalar.activation(out=l2[:], in_=l2[:], func=AF.Exp), r=["l2"], w=["l2"])
                cx.op("dve", lambda: nc.vector.scalar_tensor_tensor(out=nlam[:], in0=l2[:, 1:2], scalar=-lam_init, in1=l2[:, 0:1], op0=ALU.add, op1=ALU.subtract),
                      r=["l2"], w=["nlam"])
                cx.op("dve", lambda: nc.vector.tensor_scalar(out=gsub[:], in0=gsub[:], scalar1=(1.0 - lam_init), scalar2=None, op0=ALU.mult), r=["gsub"], w=["gsub"])

                kts = [sb(st, "ktD%d" % i, [128, T], BF16) for i in range(2)]
                vts = [sb(st, "vtD%d" % i, [128, T // 128, 128], BF16) for i in range(2)]
                qts = [sb(st, "qtD%d" % i, [128, 512], BF16) for i in range(2)]
                ets = [sb(st, "etD%d" % i, [128, 2, 512], BF16) for i in range(3)]
                zacc = sb(st, "zaccD", [128, 512], F32)
                ones_f = sb(st, "onesfD", [128, 128], F32)
                cx.op("dve", lambda: nc.vector.memset(ones_f[:], 1.0), w=["onesf"])
                r0 = sb(st, "r0D", [128, 512], F32); r1 = sb(st, "r1D", [128, 512], F32)
                o0 = sb(st, "o0D", [128, 512], F32); o1 = sb(st, "o1D", [128, 512], F32)
                av = sb(st, "avD", [128, 512], F32); a2 = sb(st, "a2D", [128, 512], BF16); rr = sb(st, "rrD", [128, 512], F32)
                yo = [sb(st, "yoD%d" % i, [128, 512], BF16) for i in range(2)]
                pS = [ps(st, "pSD%d" % i, cols=1024) for i in range(2)]
                pO = [ps(st, "pOD%d" % i) for i in range(2)]
                pZ = [ps(st, "pZD%d" % i) for i in range(2)]
                VVv = VV.rearrange("(j p) c -> p j c", p=128)
                bl = blocks512(l)
                nkt = T // 128
                et_i = 0; ps_i = 0; qi = 0
                for h in range(4):
                    kt = kts[h % 2]; vt = vts[h % 2]
                    cx.dma("sp", kt[:], KT[h * 128:(h + 1) * 128, :], r=GA("KT"), w=["ktD%d" % (h % 2)])
                    for jj in range(0, nkt, 11):
                        cx.dma("sp", vt[:, jj:jj + 11, :], VVv[:, jj:jj + 11, h * 128:(h + 1) * 128], r=GA("VV"), w=["vtD%d" % (h % 2)] if jj == 0 else (), wadd=() if jj == 0 else ["vtD%d" % (h % 2)])
                    for (t0, n) in bl:
                        isctx = t0 < CTX
                        ktiles = range(2) if isctx else range(nkt)
                        qt = qts[qi % 2]; qn = "qtD%d" % (qi % 2); qi += 1
                        cx.dma("sp", qt[:, :n], QT[h * 128:(h + 1) * 128, t0:t0 + n], r=G("QT", t0, n), w=[qn])
                        nk = len(ktiles)
                        kl = list(ktiles)
                        slots = {}

                        def emit_qk(idx):
                            nonlocal ps_i, et_i
                            j = kl[idx]
                            pS_ = pS[ps_i % 2]; psn = "pSD%d" % (ps_i % 2); ps_i += 1
                            et = ets[et_i % 3]; etn = "etD%d" % (et_i % 3); et_i += 1
                            for i in range(2):
                                cx.op("pe", lambda: nc.tensor.matmul(pS_[:, i * 512:i * 512 + n], lhsT=kt[64 * i:64 * i + 64, j * 128:(j + 1) * 128], rhs=qt[64 * i:64 * i + 64, :n], start=True, stop=True),
                                      r=["ktD%d" % (h % 2), qn], w=[psn, etn], signal=(i == 1))
                            cx.op("act", lambda: nc.scalar.activation(out=et[:, :, :n], in_=pS_[:, :].rearrange("p (i c) -> p i c", i=2)[:, :, :n], func=AF.Exp, scale=0.125), r=[psn], w=[etn])
                            slots[idx] = (et, etn)

                        def emit_pv(idx):
                            j = kl[idx]
                            et, etn = slots.pop(idx)
                            cx.op("pe", lambda: nc.tensor.matmul(pO[0][:, :n], lhsT=vt[:, j, :], rhs=et[:, 0, :n], start=(idx == 0), stop=(idx == nk - 1)),
                                  r=["vtD%d" % (h % 2), etn], w=["pOD0"], signal=False)
                            cx.op("pe", lambda: nc.tensor.matmul(pZ[0][:, :n], lhsT=ones_bf[:], rhs=et[:, 0, :n], start=(idx == 0), stop=(idx == nk - 1)),
                                  r=["ones", etn], w=["pZD0"], signal=False)
                            cx.op("pe", lambda: nc.tensor.matmul(pO[1][:, :n], lhsT=vt[:, j, :], rhs=et[:, 1, :n], start=(idx == 0), stop=(idx == nk - 1)),
                                  r=["vtD%d" % (h % 2), etn], w=["pOD1"], signal=True)
                            if idx == 0:
                                cx.op("dve", lambda: nc.vector.tensor_copy(out=zacc[:, :n], in_=et[:, 1, :n]), r=[etn], w=["zaccD"])
                            else:
                                cx.op("dve", lambda: nc.vector.tensor_tensor(out=zacc[:, :n], in0=zacc[:, :n], in1=et[:, 1, :n], op=ALU.add), r=[etn, "zaccD"], w=["zaccD"])

                        emit_qk(0)
                        for idx in range(nk):
                            if idx + 1 < nk:
                                emit_qk(idx + 1)
                            emit_pv(idx)
                        cx.op("pe", lambda: nc.tensor.matmul(pZ[1][:, :n], lhsT=ones_f[:], rhs=zacc[:, :n], start=True, stop=True), r=["onesf", "zaccD"], w=["pZD1"])
                        cx.op("dve", lambda: nc.vector.reciprocal(out=r0[:, :n], in_=pZ[0][:, :n]), r=["pZD0"], w=["r0D"])
                        cx.op("dve", lambda: nc.vector.reciprocal(out=r1[:, :n], in_=pZ[1][:, :n]), r=["pZD1"], w=["r1D"])
                        cx.op("dve", lambda: nc.vector.tensor_tensor(out=o0[:, :n], in0=pO[0][:, :n], in1=r0[:, :n], op=ALU.mult), r=["pOD0", "r0D"], w=["o0D"])
                        cx.op("dve", lambda: nc.vector.tensor_tensor(out=o1[:, :n], in0=pO[1][:, :n], in1=r1[:, :n], op=ALU.mult), r=["pOD1", "r1D"], w=["o1D"])
                        cx.op("dve", lambda: nc.vector.scalar_tensor_tensor(out=av[:, :n], in0=o1[:, :n], scalar=nlam[:, 0:1], in1=o0[:, :n], op0=ALU.mult, op1=ALU.add),
                              r=["o0D", "o1D", "nlam"], w=["avD"])
                        cx.op("dve", lambda: nc.vector.tensor_tensor(out=a2[:, :n], in0=av[:, :n], in1=av[:, :n], op=ALU.mult), r=["avD"], w=["a2D"])
                        pn = pS[ps_i % 2]; pnn = "pSD%d" % (ps_i % 2); ps_i += 1
                        cx.op("pe", lambda: nc.tensor.matmul(pn[:, :n], lhsT=ones_bf[:], rhs=a2[:, :n], start=True, stop=True), r=["ones", "a2D"], w=[pnn])
                        cx.op("act", lambda: nc.scalar.activation(out=rr[:, :n], in_=pn[:, :n], func=AF.Sqrt, scale=1.0 / 128, bias=epsc[:, 0:1]), r=[pnn, "epsc"], w=["rrD"])
                        cx.op("dve", lambda: nc.vector.reciprocal(out=rr[:, :n], in_=rr[:, :n]), r=["rrD"], w=["rrD"])
                        y = yo[qi % 2]; yn = "yoD%d" % (qi % 2)
                        cx.op("dve", lambda: nc.vector.scalar_tensor_tensor(out=y[:, :n], in0=av[:, :n], scalar=gsub[:, 0:1], in1=rr[:, :n], op0=ALU.mult, op1=ALU.mult),
                              r=["avD", "rrD", "gsub"], w=[yn])
                        cx.dma("sp", YY[0, h * 128:(h + 1) * 128, t0:t0 + n], y[:, :n], r=[yn], w=G("YY0h%d" % h, t0, n))

        def segs(l):
            return ([(0, CTX)] if l == 0 else []) + [(CTX, S)]

        def phase_E_conv(l):
            with ExitStack() as st:
                wc = sb(st, "wcE", [128, 12], F32)
                cx.dma("sp", wc[:], w_convT[l], w=["wcE"])
                sx = sb(st, "sxE", [128, T], BF16); sbb = sb(st, "sbE", [128, T], BF16); scc = sb(st, "scE", [128, T], BF16)
                u = sb(st, "uE", [128, T], F32); y = sb(st, "yE", [128, T], F32); yb = sb(st, "ybE", [128, T], BF16)
                lo = 0 if l == 0 else CTX
                for cc in range(4):
                    rows = slice(cc * 128, (cc + 1) * 128)
                    allb = list(range(17))
                    cx.dma("sp", sx[:, lo:], SXT[rows, lo:], r=GA("SXT"), w=["sxE"])
                    cx.dma("sp", sbb[:, lo:], SBT[rows, lo:], r=GA("SBT"), w=["sbE"])
                    cx.dma("sp", scc[:, lo:], SCT[rows, lo:], r=GA("SCT"), w=["scE"])
                    cx.op("dve", lambda: nc.vector.tensor_tensor(out=u[:, lo:], in0=scc[:, lo:], in1=sx[:, lo:], op=ALU.mult), r=["sxE", "scE"], w=["uE"])
                    for (a, n) in segs(l):
                        b = a + n
                        cx.op("dve", lambda: nc.vector.tensor_scalar(out=y[:, a:b], in0=u[:, a:b], scalar1=wc[:, cc * 3 + 1:cc * 3 + 2], scalar2=None, op0=ALU.mult),
                              r=["uE", "wcE"], w=["yE"])
                        cx.op("dve", lambda: nc.vector.scalar_tensor_tensor(out=y[:, a + 1:b], in0=u[:, a:b - 1], scalar=wc[:, cc * 3:cc * 3 + 1], in1=y[:, a + 1:b], op0=ALU.mult, op1=ALU.add),
                              r=["uE", "wcE", "yE"], w=["yE"])
                        cx.op("dve", lambda: nc.vector.scalar_tensor_tensor(out=y[:, a:b - 1], in0=u[:, a + 1:b], scalar=wc[:, cc * 3 + 2:cc * 3 + 3], in1=y[:, a:b - 1], op0=ALU.mult, op1=ALU.add),
                              r=["uE", "wcE", "yE"], w=["yE"])
                    cx.op("pool", lambda: nc.gpsimd.tensor_tensor(out=yb[:, lo:], in0=y[:, lo:], in1=sbb[:, lo:], op=ALU.mult), r=["yE", "sbE"], w=["ybE"])
                    cx.dma("sp", YY[1, rows, lo:], yb[:, lo:], r=["ybE"], w=["YY1_%d" % cc])

        def phase_E_pool(l):
            PAD = 16
            with ExitStack() as st:
                wp = sb(st, "wpE", [128, 4, 128], BF16); psc = sb(st, "pscE", [128, 4], F32)
                cx.dma("pool", wp[:], w_pool[l].rearrange("g c d -> c g d"), w=["wpE"])
                cx.dma("sp", psc[:], pool_scaleT[l], w=["pscE"])
                xb = sb(st, "xbP", [128, T], BF16)
                xp = sb(st, "xpP", [128, S + 2 * PAD], F32); wa = sb(st, "waP", [128, S + 2 * PAD], F32); wb = sb(st, "wbP", [128, S + 2 * PAD], F32)
                ic = sb(st, "icP", [128, T], F32); pl = sb(st, "plP", [128, T], BF16)
                yo = [sb(st, "yoP%d" % i, [128, 512], BF16) for i in range(2)]
                pq = [ps(st, "pqP%d" % i) for i in range(2)]
                lo = 0 if l == 0 else CTX
                cnt = 0
                for g in range(4):
                    rows = slice(g * 128, (g + 1) * 128)
                    cx.dma("sp", xb[:, lo:], PXT[rows, lo:], r=GA("PXT"), w=["xbP"])
                    cx.dma("sp", ic[:, lo:], invcnt_in[g:g + 1, lo:].partition_broadcast(128), w=["icP"])
                    for (a, n) in segs(l):
                        cx.op("pool", lambda: nc.gpsimd.memset(xp[:, 0:PAD], 0.0), w=["xpP"])
                        cx.op("pool", lambda: nc.gpsimd.memset(xp[:, PAD + n:PAD + n + PAD], 0.0), w=["xpP"])
                        cx.op("dve", lambda: nc.vector.tensor_copy(out=xp[:, PAD:PAD + n], in_=xb[:, a:a + n]), r=["xbP"], w=["xpP"])
                        tot = n + 2 * PAD
                        cx.op("dve", lambda: nc.vector.tensor_tensor(out=wa[:, 1:tot], in0=xp[:, 0:tot - 1], in1=xp[:, 1:tot], op=ALU.add), r=["xpP"], w=["waP"])
                        cur, curn, oth, othn = wa, "waP", wb, "wbP"
                        lo_v = 1; hi_v = tot
                        for step in range(g):
                            sft = 1 << step
                            nlo = lo_v + sft; nhi = hi_v - sft
                            cx.op("dve", lambda: nc.vector.tensor_tensor(out=oth[:, nlo:nhi], in0=cur[:, nlo - sft:nhi - sft], in1=cur[:, nlo + sft:nhi + sft], op=ALU.add),
                                  r=[curn], w=[othn])
                            cur, curn, oth, othn = oth, othn, cur, curn
                            lo_v, hi_v = nlo, nhi
                        cx.op("dve", lambda: nc.vector.tensor_tensor(out=oth[:, PAD:PAD + n], in0=cur[:, PAD:PAD + n], in1=ic[:, a:a + n], op=ALU.mult), r=[curn, "icP"], w=[othn])
                        cx.op("dve", lambda: nc.vector.tensor_tensor(out=pl[:, a:a + n], in0=oth[:, PAD:PAD + n], in1=xp[:, PAD:PAD + n], op=ALU.subtract), r=[othn, "xpP"], w=["plP"])
                    for (t0, n) in blocks512(l):
                        p = pq[cnt % 2]; pn = "pqP%d" % (cnt % 2); y = yo[cnt % 2]; yn = "yoP%d" % (cnt % 2); cnt += 1
                        cx.op("pe", lambda: nc.tensor.matmul(p[:, :n], lhsT=wp[:, g, :], rhs=pl[:, t0:t0 + n], start=True, stop=True), r=["wpE", "plP"], w=[pn])
                        cx.op("act", lambda: nc.scalar.activation(out=y[:, :n], in_=p[:, :n], func=AF.Identity, scale=psc[:, g:g + 1]), r=[pn, "pscE"], w=[yn])
                        cx.dma("sp", YY[3, rows, t0:t0 + n], y[:, :n], r=[yn], w=G("YY3g%d" % g, t0, n))

        def phase_E_fourier(l):
            with ExitStack() as st:
                cs128 = sb(st, "cs128", [128, 256], BF16); ccs = sb(st, "ccs", [128, 256], BF16); msc = sb(st, "msc", [128, 256], BF16)
                gtab = sb(st, "gtab", [64, 128 * 128], BF16)
                cx.dma("pool", cs128[:], cs128_in, w=["cs128"]); cx.dma("pool", ccs[:], ccs_in, w=["ccs"]); cx.dma("pool", msc[:], msc_in, w=["msc"])
                for q in range(4):
                    cx.dma("pool", gtab[:, q * 4096:(q + 1) * 4096], gtab_in[:, q * 4096:(q + 1) * 4096], w=["gtab"] if q == 0 else (), wadd=() if q == 0 else ["gtab"])
                gv = gtab[:].rearrange("p (k c m) -> p k c m", k=128, c=2)
                Xs = [sb(st, "XF%d" % i, [128, 64, 128], BF16) for i in range(2)]
                Xv = FX[CTX:, :].rearrange("(a b) c -> a b c", b=64)
                AT = sb(st, "ATF", [128, 128, 2, 64], BF16)
                W = sb(st, "WF", [64, 64, 256], BF16)
                YT = sb(st, "YTF", [128, S], BF16)
                pf = [ps(st, "pfF%d" % i) for i in range(4)]
                pi = 0
                for g in range(4):
                    X = Xs[g % 2]; xn = "XF%d" % (g % 2)
                    for q in range(4):
                        cx.dma("sp", X[:, q * 16:(q + 1) * 16, :], Xv[:, q * 16:(q + 1) * 16, g * 128:(g + 1) * 128], r=GA("FX"),
                               w=[xn] if q == 0 else (), wadd=() if q == 0 else [xn])
                    for n2 in range(64):
                        p = pf[pi % 4]; pn = "pfF%d" % (pi % 4); pi += 1
                        cx.op("pe", lambda: nc.tensor.matmul(p[:, 0:256], lhsT=X[:, n2, :], rhs=cs128[:], start=True, stop=True), r=[xn, "cs128"], w=[pn])
                        src = p[:, 0:256].rearrange("p (c k) -> p c k", c=2)
                        dstv = AT[:, :, :, n2].rearrange("p k c -> p c k")
                        if n2 % 2 == 0:
                            cx.op("act", lambda: nc.scalar.copy(out=dstv, in_=src), r=[pn], w=["ATF"])
                        else:
                            cx.op("dve", lambda: nc.vector.tensor_copy(out=dstv, in_=src), r=[pn], w=["ATF"])
                    for hf in range(2):
                        for kk1 in range(64):
                            k1 = hf * 64 + kk1
                            p = pf[pi % 4]; pn = "pfF%d" % (pi % 4); pi += 1
                            cx.op("pe", lambda: nc.tensor.matmul(p[0:64, 0:256], lhsT=AT[:, k1, 0, :], rhs=ccs[:], start=True, stop=False), r=["ATF", "ccs"], w=[pn], signal=False)
                            cx.op("pe", lambda: nc.tensor.matmul(p[0:64, 0:256], lhsT=AT[:, k1, 1, :], rhs=msc[:], start=False, stop=True), r=["ATF", "msc"], w=[pn])
                            if k1 % 2 == 0:
                                cx.op("act", lambda: nc.scalar.copy(out=W[:, kk1, :], in_=p[0:64, 0:256]), r=[pn], w=["WF"])
                            else:
                                cx.op("dve", lambda: nc.vector.tensor_copy(out=W[:, kk1, :], in_=p[0:64, 0:256]), r=[pn], w=["WF"])
                        for kb in range(8):
                            p = pf[pi % 4]; pn = "pfF%d" % (pi % 4); pi += 1
                            for kk in range(8):
                                kk1 = kb * 8 + kk
                                k1 = hf * 64 + kk1
                                cx.op("pe", lambda: nc.tensor.matmul(p[:, kk * 64:(kk + 1) * 64], lhsT=W[:, kk1, 0:128], rhs=gv[:, k1, 0, :], start=True, stop=False), r=["WF", "gtab"], w=[pn], signal=False)
                                cx.op("pe", lambda: nc.tensor.matmul(p[:, kk * 64:(kk + 1) * 64], lhsT=W[:, kk1, 128:256], rhs=gv[:, k1, 1, :], start=False, stop=True), r=["WF", "gtab"], w=[pn], signal=(kk == 7))
                            k1b = hf * 64 + kb * 8
                            dstv = YT[:].rearrange("p (k2 k1) -> p k1 k2", k1=128)[:, k1b:k1b + 8, :]
                            src = p[:, :].rearrange("p (a b) -> p a b", a=8)
                            if kb % 2 == 0:
                                cx.op("act", lambda: nc.scalar.copy(out=dstv, in_=src), r=[pn], w=["YTF"])
                            else:
                                cx.op("dve", lambda: nc.vector.tensor_copy(out=dstv, in_=src), r=[pn], w=["YTF"])
                    cx.dma("sp", YY[2, g * 128:(g + 1) * 128, CTX:], YT[:], r=["YTF"], w=["YY2_%d" % g])
                if l == 0:
                    cs256 = sb(st, "cs256", [128, 2, 512], BF16)
                    cx.dma("pool", cs256[:].rearrange("p a b -> p (a b)"), cs256_in, w=["cs256"])
                    Xc = sb(st, "XcF", [128, 2, 512], BF16)
                    cx.dma("sp", Xc[:], FX[0:CTX, :].rearrange("(a p) c -> p a c", p=128), r=G("FX", 0, CTX), w=["XcF"])
                    AB = sb(st, "ABF", [128, 512], BF16); yc = sb(st, "ycF", [128, 256], BF16)
                    sc_ = 1.0 / math.sqrt(256.0 * 128.0)
                    for g in range(4):
                        p = pf[pi % 4]; pn = "pfF%d" % (pi % 4); pi += 1
                        for a in range(2):
                            cx.op("pe", lambda: nc.tensor.matmul(p[:, :], lhsT=Xc[:, a, g * 128:(g + 1) * 128], rhs=cs256[:, a, :], start=(a == 0), stop=(a == 1)), r=["XcF", "cs256"], w=[pn], signal=(a == 1))
                        cx.op("dve", lambda: nc.vector.tensor_copy(out=AB[:], in_=p[:, :]), r=[pn], w=["ABF"])
                        p2 = pf[pi % 4]; pn2 = "pfF%d" % (pi % 4); pi += 1
                        cx.op("pe", lambda: nc.tensor.matmul(p2[:, 0:256], lhsT=ccs[:, 0:128], rhs=AB[:, 0:256], start=True, stop=False), r=["ccs", "ABF"], w=[pn2], signal=False)
                        cx.op("pe", lambda: nc.tensor.matmul(p2[:, 0:256], lhsT=msc[:, 0:128], rhs=AB[:, 256:512], start=False, stop=True), r=["msc", "ABF"], w=[pn2])
                        cx.op("act", lambda: nc.scalar.activation(out=yc[:], in_=p2[:, 0:256], func=AF.Copy, scale=sc_), r=[pn2], w=["ycF"])
                        cx.dma("sp", YY[2, g * 128:(g + 1) * 128, 0:CTX], yc[:], r=["ycF"], w=["YY2c_%d" % g])

        def phase_F(l):
            with ExitStack() as st:
                wbr = sb(st, "wbr", [128, 16, 1024], BF16); wmg = sb(st, "wmg", [128, 8, 4096], BF16); wo = sb(st, "woF", [128, 8, 1024], BF16)
                bmg = sb(st, "bmg", [128, 32], F32)
                cx.dma("sp", bmg[:], b_mgateT[l], w=["bmg"])
                wbv = w_branch[l].rearrange("b (c p) n -> p (b c) n", p=128)
                for q in range(16):
                    cx.dma("pool", wbr[:, q, :], wbv[:, q, :], w=["wbr"] if q == 0 else (), wadd=() if q == 0 else ["wbr"])
                wmv = w_mgate[l].rearrange("(k p) n -> p k n", p=128)
                for k in range(8):
                    cx.dma("pool", wmg[:, k, :], wmv[:, k, :], w=["wmg"] if k == 0 else (), wadd=() if k == 0 else ["wmg"])
                wov = w_out[l].rearrange("(k p) n -> p k n", p=128)
                for k in range(8):
                    cx.dma("pool", wo[:, k, :], wov[:, k, :], w=["woF"] if k == 0 else (), wadd=() if k == 0 else ["woF"])
                NB = 256
                hTs = [sb(st, "hTF%d" % i, [128, 8, NB], BF16) for i in range(2)]
                ys = [sb(st, "yF%d" % i, [128, 16, NB], BF16) for i in range(2)]
                xts = [sb(st, "xtF%d" % i, [128, 8, NB], F32) for i in range(2)]
                sacc = sb(st, "saccF", [128, 8, NB], F32); sT = sb(st, "sTF", [128, 8, NB], BF16)
                sig = [sb(st, "sigF%d" % i, [128, NB], F32) for i in range(2)]
                tm = [sb(st, "tmF%d" % i, [128, NB], F32) for i in range(2)]
                pG = [ps(st, "pGF%d" % i) for i in range(2)]
                pP = [ps(st, "pPF%d" % i) for i in range(2)]
                pW = [ps(st, "pWF%d" % i) for i in range(2)]
                lo = 0 if l == 0 else CTX
                YYv = YY.rearrange("b (c p) t -> p (b c) t", p=128)
                ci = 0
                for bi, t0 in enumerate(range(lo, T, NB)):
                    n = NB
                    mi = 1 if t0 < CTX else 0
                    hT = hTs[bi % 2]; y = ys[bi % 2]; xt = xts[bi % 2]; tg = "F%d" % (bi % 2)
                    yr = []
                    for q4 in range(4):
                        yr += G("YY0h%d" % q4, t0, n) + G("YY3g%d" % q4, t0, n) + ["YY1_%d" % q4, "YY2_%d" % q4, "YY2c_%d" % q4]
                    cx.dma("sp", hT[:], HTv[:, :, t0:t0 + n], r=G("HT", t0, n), w=["hT" + tg])
                    cx.dma("sp", y[:], YYv[:, :, t0:t0 + n], r=yr, w=["y" + tg])
                    cx.dma("sp", xt[:], XTv[:, :, t0:t0 + n], r=G("XT", t0, n), w=["xt" + tg])
                    for dc in range(8):
                        for br in range(4):
                            g_ = pG[ci % 2]; gn = "pGF%d" % (ci % 2); p_ = pP[ci % 2]; pn = "pPF%d" % (ci % 2)
                            sg_ = sig[ci % 2]; sgn = "sigF%d" % (ci % 2); tm_ = tm[ci % 2]; tmn = "tmF%d" % (ci % 2); ci += 1
                            col = br * 1024 + dc * 128
                            for k in range(8):
                                cx.op("pe", lambda: nc.tensor.matmul(g_[:, :n], lhsT=wmg[:, k, col:col + 128], rhs=hT[:, k, :], start=(k == 0), stop=(k == 7)),
                                      r=["wmg", "hT" + tg], w=[gn], signal=(k == 7))
                            for c in range(4):
                                cx.op("pe", lambda: nc.tensor.matmul(p_[:, :n], lhsT=wbr[:, br * 4 + c, dc * 128:(dc + 1) * 128], rhs=y[:, br * 4 + c, :], start=(c == 0), stop=(c == 3)),
                                      r=["wbr", "y" + tg], w=[pn], signal=(c == 3))
                            cx.op("act", lambda: nc.scalar.activation(out=sg_[:, :n], in_=g_[:, :n], func=AF.Sigmoid, bias=bmg[:, br * 8 + dc:br * 8 + dc + 1], scale=1.0),
                                  r=[gn, "bmg"], w=[sgn])
                            if br == 0:
                                cx.op("dve", lambda: nc.vector.tensor_tensor(out=sacc[:, dc, :], in0=p_[:, :n], in1=sg_[:, :n], op=ALU.mult), r=[pn, sgn], w=["saccF"])
                            else:
                                cx.op("dve", lambda: nc.vector.tensor_tensor(out=tm_[:, :n], in0=p_[:, :n], in1=sg_[:, :n], op=ALU.mult), r=[pn, sgn], w=[tmn])
                                if br < 3:
                                    cx.op("pool", lambda: nc.gpsimd.tensor_tensor(out=sacc[:, dc, :], in0=sacc[:, dc, :], in1=tm_[:, :n], op=ALU.add), r=["saccF", tmn], w=["saccF"])
                                else:
                                    cx.op("pool", lambda: nc.gpsimd.tensor_tensor(out=sT[:, dc, :], in0=sacc[:, dc, :], in1=tm_[:, :n], op=ALU.add), r=["saccF", tmn], w=["sTF"])
                    for dc in range(8):
                        w_ = pW[dc % 2]; wn = "pWF%d" % (dc % 2)
                        for k in range(8):
                            cx.op("pe", lambda: nc.tensor.matmul(w_[:, :n], lhsT=wo[:, k, dc * 128:(dc + 1) * 128], rhs=sT[:, k, :], start=(k == 0), stop=(k == 7)),
                                  r=["woF", "sTF"], w=[wn], signal=(k == 7))
                        cx.op("dve", lambda: nc.vector.scalar_tensor_tensor(out=xt[:, dc, :], in0=w_[:, :n], scalar=gt1[:, mi, dc:dc + 1], in1=xt[:, dc, :], op0=ALU.mult, op1=ALU.add),
                              r=[wn, "modv", "xt" + tg], w=["xt" + tg])
                    cx.dma("sp", XTv[:, :, t0:t0 + n], xt[:], r=["xt" + tg], w=G("XT", t0, n))

        def phase_G(l):
            with ExitStack() as st:
                wr = sb(st, "wrG", [128, 8, NE], F32); br_ = sb(st, "brG", [128, NE], F32)
                cx.dma("sp", wr[:], w_router[l].rearrange("(k p) e -> p k e", p=128), w=["wrG"])
                cx.dma("sp", br_[:], b_router_rep[l], w=["brG"])
                xts = [sb(st, "xtG%d" % i, [128, 8, 512], F32) for i in range(2)]
                hTs = [sb(st, "hTG%d" % i, [128, 8, 512], BF16) for i in range(2)]
                h32 = sb(st, "h32G", [128, 8, 512], F32)
                sq = sb(st, "sqG", [128, 8, 512], BF16); R = sb(st, "RG", [128, 512], F32); tmp = sb(st, "tmpG", [128, 8, 512], F32)
                lg = sb(st, "lgG", [128, NE], F32); m8 = sb(st, "m8G", [128, 8], F32); msk = sb(st, "mskG", [128, NE], F32)
                nm = sb(st, "nmG", [128, 1], F32); ex = sb(st, "exG", [128, NE], F32); sm = sb(st, "smG", [128, 1], F32)
                cb = sb(st, "cbG", [128, NE], F32); cT = [sb(st, "cTG%d" % i, [NE, 512], F32) for i in range(2)]
                pss = ps(st, "pssG"); pl_ = [ps(st, "plG%d" % i) for i in range(2)]; pt_ = ps(st, "ptG")
                for bi, (t0, n) in enumerate(blocks512(l)):
                    mi = 1 if t0 < CTX else 0
                    xt = xts[bi % 2]; hT = hTs[bi % 2]; tg = "G%d" % (bi % 2)
                    cx.dma("sp", xt[:, :, :n], XTv[:, :, t0:t0 + n], r=G("XT", t0, n), w=["xt" + tg])
                    norm_block((sq, pss, R, tmp), xt, n, gs2, sh2, mi, hT, h32=h32, tag=tg)
                    cx.dma("sp", H2Tv[:, :, t0:t0 + n], hT[:, :, :n], r=["hT" + tg], w=G("H2T", t0, n))
                    ct = cT[bi % 2]; ctn = "cTG%d" % (bi % 2)
                    for j in range(n // 128):
                        p = pl_[j % 2]; pn = "plG%d" % (j % 2)
                        for k in range(8):
                            cx.op("pe", lambda: nc.tensor.matmul(p[:, 0:NE], lhsT=h32[:, k, j * 128:(j + 1) * 128], rhs=wr[:, k, :], start=(k == 0), stop=(k == 7)),
                                  r=["h32", "wrG"], w=[pn], signal=(k == 7))
                        cx.op("dve", lambda: nc.vector.tensor_tensor(out=lg[:], in0=p[:, 0:NE], in1=br_[:], op=ALU.add), r=[pn, "brG"], w=["lgG"])
                        cx.op("dve", lambda: nc.vector.max(out=m8[:], in_=lg[:]), r=["lgG"], w=["m8G"])
                        cx.op("dve", lambda: nc.vector.tensor_scalar(out=msk[:], in0=lg[:], scalar1=m8[:, 3:4], scalar2=None, op0=ALU.is_ge), r=["lgG", "m8G"], w=["mskG"])
                        cx.op("dve", lambda: nc.vector.tensor_scalar(out=nm[:], in0=m8[:, 0:1], scalar1=-1.0, scalar2=None, op0=ALU.mult), r=["m8G"], w=["nmG"])
                        cx.op("act", lambda: nc.scalar.activation(out=ex[:], in_=lg[:], func=AF.Exp, bias=nm[:, 0:1], scale=1.0), r=["lgG", "nmG"], w=["exG"])
                        cx.op("dve", lambda: nc.vector.tensor_tensor(out=ex[:], in0=ex[:], in1=msk[:], op=ALU.mult), r=["exG", "mskG"], w=["exG"])
                        cx.op("dve", lambda: nc.vector.reduce_sum(out=sm[:], in_=ex[:], axis=AX.X), r=["exG"], w=["smG"])
                        cx.op("dve", lambda: nc.vector.reciprocal(out=sm[:], in_=sm[:]), r=["smG"], w=["smG"])
                        cx.op("dve", lambda: nc.vector.tensor_scalar(out=cb[:], in0=ex[:], scalar1=sm[:, 0:1], scalar2=None, op0=ALU.mult), r=["exG", "smG"], w=["cbG"])
                        cx.op("pe", lambda: nc.tensor.transpose(pt_[0:NE, 0:128], cb[:], ident[:]), r=["cbG", "ident"], w=["ptG"])
                        cx.op("act", lambda: nc.scalar.copy(out=ct[:, j * 128:(j + 1) * 128], in_=pt_[0:NE, 0:128]), r=["ptG"], w=[ctn])
                    cx.dma("sp", COMBT[:, t0:t0 + n], ct[:, :n], r=[ctn], w=G("COMBT", t0, n))

        def phase_H(l):
            with ExitStack() as st:
                bg = sb(st, "bgH", [128, NE * 8], F32); bu = sb(st, "buH", [128, NE * 8], F32); bd = sb(st, "bdH", [NE, D], F32)
                cx.dma("sp", bg[:], b_egT[l], w=["bgH"]); cx.dma("sp", bu[:], b_euT[l], w=["buH"]); cx.dma("sp", bd[:], b_ed[l], w=["bdH"])
                cx.op("dve", lambda: nc.vector.tensor_scalar(out=bu[:], in0=bu[:], scalar1=1.0, scalar2=None, op0=ALU.add), r=["buH"], w=["buH"])
                SBK = 1024
                wg = [sb(st, "wgH%d" % i, [128, 8, D], BF16) for i in range(2)]
                wu = [sb(st, "wuH%d" % i, [128, 8, D], BF16) for i in range(2)]
                wd = [sb(st, "wdH%d" % i, [128, 8, D], BF16) for i in range(2)]
                h2s = [sb(st, "h2H%d" % i, [128, 8, SBK], BF16) for i in range(2)]; acc = sb(st, "accH", [128, 8, SBK], F32)
                cT = sb(st, "cTH", [NE, SBK], F32)
                aT = [sb(st, "aTH%d" % i, [128, 8, 512], BF16) for i in range(2)]
                cB = [sb(st, "cBH%d" % i, [128, 512], F32) for i in range(2)]
                gt = [sb(st, "gtH%d" % i, [128, 512], F32) for i in range(2)]
                yt_ = [sb(st, "ytH%d" % i, [128, 512], F32) for i in range(2)]
                ut = [sb(st, "utH%d" % i, [128, 512], F32) for i in range(2)]
                pG = [ps(st, "pGH%d" % i) for i in range(3)]
                pU = [ps(st, "pUH%d" % i) for i in range(3)]
                pY = [ps(st, "pYH%d" % i) for i in range(2)]
                lo = 0 if l == 0 else CTX
                sblocks = []
                t = lo
                while t < T:
                    n = min(SBK, T - t); sblocks.append((t, n)); t += n
                ld = 0
                wgv = lambda e: w_eg[l, e].rearrange("(k p) n -> p k n", p=128)
                wuv = lambda e: w_eu[l, e].rearrange("(k p) n -> p k n", p=128)
                wdv = lambda e: w_ed[l, e].rearrange("(k p) n -> p k n", p=128)

                pend = []

                def sched_loads(e, slot):
                    for (wt, wv_, nm, kind) in ((wg, wgv, "wgH", "gu"), (wu, wuv, "wuH", "gu"), (wd, wdv, "wdH", "d")):
                        for k in range(0, 8, 4):
                            pend.append((kind, (lambda wt=wt, wv_=wv_, nm=nm, k=k: cx.dma(
                                "pool", wt[slot][:, k:k + 4, :], wv_(e)[:, k:k + 4, :],
                                w=["%s%d" % (nm, slot)] if k == 0 else (), wadd=() if k == 0 else ["%s%d" % (nm, slot)]))))

                def pump(allow_d):
                    if pend and (pend[0][0] == "gu" or allow_d):
                        pend.pop(0)[1]()

                def load_w(e, slot):
                    for k in range(0, 8, 4):
                        cx.dma("pool", wg[slot][:, k:k + 4, :], wgv(e)[:, k:k + 4, :], w=["wgH%d" % slot] if k == 0 else (), wadd=() if k == 0 else ["wgH%d" % slot])
                    for k in range(0, 8, 4):
                        cx.dma("pool", wu[slot][:, k:k + 4, :], wuv(e)[:, k:k + 4, :], w=["wuH%d" % slot] if k == 0 else (), wadd=() if k == 0 else ["wuH%d" % slot])
                    for k in range(0, 8, 4):
                        cx.dma("pool", wd[slot][:, k:k + 4, :], wdv(e)[:, k:k + 4, :], w=["wdH%d" % slot] if k == 0 else (), wadd=() if k == 0 else ["wdH%d" % slot])

                gi = 0
                fi = 0
                cur_slot = 0
                prev = None
                first = True
                xbufs = [(gt[0], "gtH0"), (gt[1], "gtH1"), (ut[0], "utH0"), (ut[1], "utH1")]
                xi_ = 0
                nsb = len(sblocks)
                for sbi, (s0, sn) in enumerate(sblocks):
                    h2 = h2s[sbi % 2]; h2n = "h2H%d" % (sbi % 2)
                    if sbi == 0:
                        cx.dma("sp", h2[:, :, :sn], H2Tv[:, :, s0:s0 + sn], r=G("H2T", s0, sn), w=[h2n])
                    cx.dma("sp", cT[:, :sn], COMBT[:, s0:s0 + sn], r=G("COMBT", s0, sn), w=["cTH"])
                    subs = [(q, min(512, sn - q)) for q in range(0, sn, 512)]
                    for (q, n) in subs:
                        for dc in range(8):
                            pB = pY[dc % 2]; pBn = "pYH%d" % (dc % 2)
                            cx.op("pe", lambda: nc.tensor.matmul(pB[:, :n], lhsT=bd[:, dc * 128:(dc + 1) * 128], rhs=cT[:, q:q + n], start=True, stop=True), r=["bdH", "cTH"], w=[pBn])
                            cx.op("act", lambda: nc.scalar.copy(out=acc[:, dc, q:q + n], in_=pB[:, :n]), r=[pBn], w=["accH%d" % dc])
                    work = [(e, q, n) for e in range(NE) for (q, n) in subs]
                    if first:
                        load_w(0, cur_slot)

                    def emit_GU(e, q, n, slot, par, allow_d=False):
                        nonlocal fi
                        a = aT[par]; an = "aTH%d" % par
                        cb_ = cB[par]; cbn = "cBH%d" % par
                        cx.dma("sp", cb_[:, :n], COMBT[e:e + 1, s0 + q:s0 + q + n].partition_broadcast(128), r=G("COMBT", s0 + q, n), w=[cbn])
                        for fc in range(8):
                            g_ = pG[fi % 3]; gn = "pGH%d" % (fi % 3); u_ = pU[fi % 3]; un = "pUH%d" % (fi % 3)
                            g1_ = gt[fi % 2]; g1n = "gtH%d" % (fi % 2); s1_ = g1_; s1n = g1n; u1_ = ut[fi % 2]; u1n = "utH%d" % (fi % 2)
                            fi += 1
                            for k in range(8):
                                cx.op("pe", lambda: nc.tensor.matmul(g_[:, :n], lhsT=wg[slot][:, k, fc * 128:(fc + 1) * 128], rhs=h2[:, k, q:q + n], start=(k == 0), stop=(k == 7)),
                                      r=["wgH%d" % slot, h2n], w=[gn], signal=(k == 7))
                            for k in range(8):
                                cx.op("pe", lambda: nc.tensor.matmul(u_[:, :n], lhsT=wu[slot][:, k, fc * 128:(fc + 1) * 128], rhs=h2[:, k, q:q + n], start=(k == 0), stop=(k == 7)),
                                      r=["wuH%d" % slot, h2n], w=[un], signal=(k == 7))
                            bi_ = e * 8 + fc
                            cx.op("dve", lambda: nc.vector.tensor_scalar(out=g1_[:, :n], in0=g_[:, :n], scalar1=bg[:, bi_:bi_ + 1], scalar2=LIMIT, op0=ALU.add, op1=ALU.min), r=[gn, "bgH"], w=[g1n])
                            cx.op("act", lambda: nc.scalar.activation(out=s1_[:, :n], in_=g1_[:, :n], func=AF.Gelu_apprx_sigmoid), r=[g1n], w=[g1n])
                            cx.op("dve", lambda: nc.vector.tensor_scalar(out=u1_[:, :n], in0=u_[:, :n], scalar1=bu[:, bi_:bi_ + 1], scalar2=LIMIT + 1.0, op0=ALU.add, op1=ALU.min), r=[un, "buH"], w=[u1n])
                            cx.op("dve", lambda: nc.vector.scalar_tensor_tensor(out=u1_[:, :n], in0=u1_[:, :n], scalar=1.0 - LIMIT, in1=s1_[:, :n], op0=ALU.max, op1=ALU.mult), r=[u1n, s1n], w=[u1n])
                            cx.op("pool", lambda: nc.gpsimd.tensor_tensor(out=a[:, fc, :n], in0=u1_[:, :n], in1=cb_[:, :n], op=ALU.mult), r=[u1n, cbn], w=[an])
                            if fc % 2 == 1:
                                pump(allow_d)

                    def emit_Y(e, q, n, slot, par):
                        a = aT[par]; an = "aTH%d" % par
                        for dc in range(8):
                            y_ = pY[dc % 2]; yn = "pYH%d" % (dc % 2)
                            for fc in range(8):
                                cx.op("pe", lambda: nc.tensor.matmul(y_[:, :n], lhsT=wd[slot][:, fc, dc * 128:(dc + 1) * 128], rhs=a[:, fc, :n], start=(fc == 0), stop=(fc == 7)),
                                      r=["wdH%d" % slot, an], w=[yn], signal=(fc == 7))
                            if dc % 2 == 0:
                                cx.op("dve", lambda: nc.vector.tensor_tensor(out=acc[:, dc, q:q + n], in0=y_[:, :n], in1=acc[:, dc, q:q + n], op=ALU.add), r=[yn, "accH%d" % dc], w=["accH%d" % dc])
                            else:
                                yt = yt_[(dc // 2) % 2]; ytn = "ytH%d" % ((dc // 2) % 2)
                                cx.op("act", lambda: nc.scalar.copy(out=yt[:, :n], in_=y_[:, :n]), r=[yn], w=[ytn])
                                cx.op("pool", lambda: nc.gpsimd.tensor_tensor(out=acc[:, dc, q:q + n], in0=yt[:, :n], in1=acc[:, dc, q:q + n], op=ALU.add), r=[ytn, "accH%d" % dc], w=["accH%d" % dc])

                    for wi, (e, q, n) in enumerate(work):
                        if q == 0 and not first:
                            cur_slot = 1 - cur_slot
                        par = gi % 2; gi += 1
                        if q == 0:
                            while pend:
                                pend.pop(0)[1]()
                            if e + 1 < NE:
                                sched_loads(e + 1, 1 - cur_slot)
                            elif sbi + 1 < nsb:
                                sched_loads(0, 1 - cur_slot)
                            if e == 12 and sbi + 1 < nsb:
                                (s0n, snn) = sblocks[sbi + 1]
                                cx.dma("sp", h2s[(sbi + 1) % 2][:, :, :snn], H2Tv[:, :, s0n:s0n + snn], r=G("H2T", s0n, snn), w=["h2H%d" % ((sbi + 1) % 2)])
                        first = False
                        emit_GU(e, q, n, cur_slot, par, allow_d=(q != 0))
                        if prev is not None:
                            emit_Y(*prev)
                        prev = (e, q, n, cur_slot, par)
                        if len(subs) == 1:
                            while pend:
                                pend.pop(0)[1]()
                    emit_Y(*prev)
                    prev = None
                    pieces = []
                    for (q, n) in subs:
                        t0 = s0 + q
                        if t0 < CTX < t0 + n:
                            pieces += [(q, CTX - t0), (q + CTX - t0, n - (CTX - t0))]
                        else:
                            pieces.append((q, n))
                    for (q, n) in pieces:
                        t0 = s0 + q
                        mi = 1 if t0 < CTX else 0
                        for dg in range(0, 8, 4):
                            for dc in range(dg, dg + 4):
                                xr, xrn = xbufs[dc - dg]
                                cx.dma("sp", xr[:, :n], XT[dc * 128:(dc + 1) * 128, t0:t0 + n], r=G("XT", t0, n), w=[xrn])
                            for dc in range(dg, dg + 4):
                                xr, xrn = xbufs[dc - dg]
                                cx.op("dve", lambda: nc.vector.scalar_tensor_tensor(out=xr[:, :n], in0=acc[:, dc, q:q + n], scalar=gt2[:, mi, dc:dc + 1], in1=xr[:, :n], op0=ALU.mult, op1=ALU.add),
                                      r=["accH%d" % dc, "modv", xrn], w=[xrn])
                            for dc in range(dg, dg + 4):
                                xr, xrn = xbufs[dc - dg]
                                cx.dma("sp", XT[dc * 128:(dc + 1) * 128, t0:t0 + n], xr[:, :n], r=[xrn], w=G("XT", t0, n) if dc == 0 else (), wadd=() if dc == 0 else G("XT", t0, n))
                while pend:
                    pend.pop(0)[1]()

        def phase_I():
            with ExitStack() as st:
                gf = sb(st, "gfI", [128, 8], F32)
                cx.dma("sp", gf[:], gfT, w=["gfI"])
                xts = [sb(st, "xtI%d" % i, [128, 8, 512], F32) for i in range(2)]
                sq = sb(st, "sqI", [128, 8, 512], BF16); R = sb(st, "RI", [128, 512], F32); yt = sb(st, "ytI", [128, 8, 512], F32)
                oo = [sb(st, "ooI%d" % i, [128, D], F32) for i in range(2)]
                pss = ps(st, "pssI"); pt = [ps(st, "ptI%d" % i) for i in range(4)]
                oi = 0
                for bi, (t0, n) in enumerate(blocks512(1)):
                    xt = xts[bi % 2]; tg = "I%d" % (bi % 2)
                    cx.dma("sp", xt[:, :, :n], XTv[:, :, t0:t0 + n], r=G("XT", t0, n), w=["xt" + tg])
                    for k in range(8):
                        cx.op("dve", lambda: nc.vector.tensor_tensor(out=sq[:, k, :n], in0=xt[:, k, :n], in1=xt[:, k, :n], op=ALU.mult), r=["xt" + tg], w=["sq"])
                    for k in range(8):
                        cx.op("pe", lambda: nc.tensor.matmul(pss[:, :n], lhsT=ones_bf[:], rhs=sq[:, k, :n], start=(k == 0), stop=(k == 7)), r=["sq", "ones"], w=["pss"], signal=(k == 7))
                    cx.op("act", lambda: nc.scalar.activation(out=R[:, :n], in_=pss[:, :n], func=AF.Sqrt, scale=1.0 / D, bias=epsc[:, 0:1]), r=["pss", "epsc"], w=["R"])
                    cx.op("dve", lambda: nc.vector.reciprocal(out=R[:, :n], in_=R[:, :n]), r=["R"], w=["R"])
                    for k in range(8):
                        cx.op("dve", lambda: nc.vector.scalar_tensor_tensor(out=yt[:, k, :n], in0=xt[:, k, :n], scalar=gf[:, k:k + 1], in1=R[:, :n], op0=ALU.mult, op1=ALU.mult),
                              r=["xt" + tg, "R", "gfI"], w=["ytI"])
                    for j in range(n // 128):
                        o = oo[oi % 2]; on = "ooI%d" % (oi % 2); oi += 1
                        for hf in range(2):
                            p = pt[(j % 2) * 2 + hf]; pn = "ptI%d" % ((j % 2) * 2 + hf)
                            for q in range(4):
                                k = hf * 4 + q
                                cx.op("pe", lambda: nc.tensor.transpose(p[:, q * 128:(q + 1) * 128], yt[:, k, j * 128:(j + 1) * 128], ident[:]), r=["ytI", "ident"], w=[pn], signal=(q == 3))
                            if hf == 0:
                                cx.op("act", lambda: nc.scalar.copy(out=o[:, 0:512], in_=p[:, :]), r=[pn], w=[on + "a"])
                            else:
                                cx.op("dve", lambda: nc.vector.tensor_copy(out=o[:, 512:1024], in_=p[:, :]), r=[pn], w=[on + "b"])
                        r0_ = t0 - CTX + j * 128
                        cx.dma("sp", out_ap[r0_:r0_ + 128, :], o[:], r=[on + "a", on + "b"], wadd=["OUT"])

        order = []
        order.append(("B", phase_B))
        for l in range(layers):
            order += [("A%d" % l, lambda l=l: phase_A(l)), ("C%d" % l, lambda l=l: phase_C(l)), ("D%d" % l, lambda l=l: phase_D(l)),
                      ("Ec%d" % l, lambda l=l: phase_E_conv(l)), ("Ep%d" % l, lambda l=l: phase_E_pool(l)), ("Ef%d" % l, lambda l=l: phase_E_fourier(l)),
                      ("F%d" % l, lambda l=l: phase_F(l)), ("G%d" % l, lambda l=l: phase_G(l)), ("H%d" % l, lambda l=l: phase_H(l))]
        order.append(("I", phase_I))
        for name, fn in order:
            cx.barrier()
            fn()
            if stop_after is not None and name == stop_after:
                break
        cx.finish()
    return nc


def _colT(v, nchunk):
    return np.ascontiguousarray(np.swapaxes(v.reshape(v.shape[:-1] + (nchunk, 128)), -1, -2))


def make_in_maps(inputs, n_cores=8):
    f = lambda a: np.ascontiguousarray(np.asarray(a, dtype=np.float32))
    x = f(inputs["x"]); c = f(inputs["c"]); ctx = f(inputs["ctx"]); c_ctx = f(inputs["c_ctx"])
    w_in = f(inputs["w_in"])
    perm = _rot_perm()
    cols = []
    for which in range(2):
        for h in range(4):
            for i in range(2):
                base = which * 512 + h * 128 + i * 64
                cols.append(base + perm)
    cols = np.concatenate(cols)
    w_in_rot = np.ascontiguousarray(w_in[:, :, cols])
    cosT, sinT = _rope_tables()
    cs128, ccs, msc, g, cs256 = _dft_tables()
    shared = {
        "w_ada": f(inputs["w_ada"]), "b_adaT": _colT(f(inputs["b_ada"]), 48),
        "g1T": _colT(f(inputs["g_norm1"]), 8), "g2T": _colT(f(inputs["g_norm2"]), 8), "gfT": _colT(f(inputs["g_final"]), 8),
        "w_in": w_in, "w_in_rot": w_in_rot,
        "lam_rep": np.ascontiguousarray(np.broadcast_to(f(inputs["da_lambda"]).reshape(L, 1, 256), (L, 128, 256))),
        "gsubT": np.ascontiguousarray(f(inputs["g_subln"]).reshape(L, 128, 1)),
        "w_convT": np.ascontiguousarray(f(inputs["w_conv"]).reshape(L, 3, 4, 128).transpose(0, 3, 2, 1).reshape(L, 128, 12)),
        "w_pool": f(inputs["w_pool"]), "pool_scaleT": _colT(f(inputs["pool_scale"]), 4),
        "w_branch": f(inputs["w_branch"]), "w_mgate": f(inputs["w_mgate"]), "b_mgateT": _colT(f(inputs["b_mgate"]), 32),
        "w_out": f(inputs["w_out"]), "w_router": f(inputs["w_router"]),
        "b_router_rep": np.ascontiguousarray(np.broadcast_to(f(inputs["b_router"]).reshape(L, 1, NE), (L, 128, NE))),
        "w_e_gate": f(inputs["w_e_gate"]), "w_e_up": f(inputs["w_e_up"]), "w_e_down": f(inputs["w_e_down"]),
        "b_egT": np.ascontiguousarray(_colT(f(inputs["b_e_gate"]), 8).transpose(0, 2, 1, 3).reshape(L, 128, NE * 8)),
        "b_euT": np.ascontiguousarray(_colT(f(inputs["b_e_up"]), 8).transpose(0, 2, 1, 3).reshape(L, 128, NE * 8)),
        "b_e_down": f(inputs["b_e_down"]),
        "rope_cos": cosT, "rope_sin": sinT, "ident": np.eye(128, dtype=np.float32),
        "cs128": cs128, "ccs": ccs, "msc": msc, "gtab": g, "cs256": cs256, "invcnt": _invcnt_tables(),
    }
    maps = []
    for b in range(n_cores):
        m = dict(shared)
        m["x"] = x[b]; m["ctx"] = ctx[b]
        m["cvec"] = np.ascontiguousarray(np.concatenate([_colT(c[b], 8), _colT(c_ctx, 8)], axis=1))
        maps.append(m)
    return maps


def kernel(**inputs):
    nc = build_program()
    maps = make_in_maps(inputs, 8)
    res = run_bass_kernel_spmd(nc, maps, core_ids=list(range(8)))
    return np.stack([np.asarray(r["out"], dtype=np.float32) for r in res.results], axis=0)
```
